# Optimizing a Trainium2 kernel written in Bass

```python
import math
import jax, jax.numpy as jnp
from jax import lax
import numpy as np

D_MODEL = 1024
BATCH = 16
SEQ = 2048
DEPTH = 2

MLSTM_HEADS = 4
MLSTM_DH = 128
MLSTM_W = MLSTM_HEADS * MLSTM_DH
MLSTM_CHUNK = 128
CONV_W = 5
N_MGATES = 4 * MLSTM_HEADS

DIFF_HEADS = 4
DIFF_DH = 64
DIFF_DV = 2 * DIFF_DH
DIFF_QK_W = DIFF_HEADS * 2 * DIFF_DH
DIFF_W = DIFF_HEADS * DIFF_DV
Q_BLOCK = 128

OFF_MQ = 0
OFF_MK = OFF_MQ + MLSTM_W
OFF_MV = OFF_MK + MLSTM_W
OFF_MO = OFF_MV + MLSTM_W
OFF_MG = OFF_MO + MLSTM_W
OFF_DQ = OFF_MG + N_MGATES
OFF_DK = OFF_DQ + DIFF_QK_W
OFF_DV = OFF_DK + DIFF_QK_W
OFF_GATE = OFF_DV + DIFF_W
N_BRANCH = 2
IN_COLS = OFF_GATE + N_BRANCH * D_MODEL

N_EXPERTS = 16
EC_CAPACITY = 2
D_FF_EXPERT = 2 * D_MODEL

EPS = 1e-6

kernel_name = "hybrid_mlstm_diffattn_ecmoe_encoder"


def rms_norm(x, g):
    xf = x.astype(jnp.float32)
    xf = xf * lax.rsqrt(jnp.mean(xf * xf, axis=-1, keepdims=True) + EPS)
    return (xf * g.astype(jnp.float32)).astype(x.dtype)


def centred_dwconv(x, w, b):
    c = x.shape[-1]
    pad = CONV_W // 2
    y = lax.conv_general_dilated(x, w[:, None, :].astype(x.dtype), window_strides=(1,),
                                 padding=[(pad, pad)], dimension_numbers=('NWC', 'WIO', 'NWC'),
                                 feature_group_count=c)
    return y + b.astype(x.dtype)


def mlstm_chunkwise(q, k, v, i_pre, f_pre):
    B, H, S, dk = q.shape
    dv = v.shape[-1]
    L = MLSTM_CHUNK
    NC = S // L
    q = q.astype(jnp.float32).reshape(B, H, NC, L, dk)
    k = (k.astype(jnp.float32) * dk ** -0.5).reshape(B, H, NC, L, dk)
    v = v.astype(jnp.float32).reshape(B, H, NC, L, dv)
    log_f = jax.nn.log_sigmoid(f_pre).reshape(B, H, NC, L)
    log_i = i_pre.reshape(B, H, NC, L)
    b = jnp.cumsum(log_f, axis=-1)
    g = b[..., -1]
    a = g[..., None] - b + log_i
    a_max = a.max(-1)
    w = jnp.exp(a - a_max[..., None])
    C_loc = jnp.einsum('bhclv,bhclk->bhcvk', v * w[..., None], k)
    n_loc = jnp.einsum('bhcl,bhclk->bhck', w, k)

    def step(carry, inp):
        C, n, m = carry
        C_l, n_l, am, gc = inp
        m_new = jnp.maximum(gc + m, am)
        sp = jnp.exp(gc + m - m_new)
        sl = jnp.exp(am - m_new)
        C_new = sp[..., None, None] * C + sl[..., None, None] * C_l
        n_new = sp[..., None] * n + sl[..., None] * n_l
        return (C_new, n_new, m_new), (C, n, m)

    xs = (jnp.moveaxis(C_loc, 2, 0), jnp.moveaxis(n_loc, 2, 0),
          jnp.moveaxis(a_max, 2, 0), jnp.moveaxis(g, 2, 0))
    init = (jnp.zeros((B, H, dv, dk), jnp.float32), jnp.zeros((B, H, dk), jnp.float32),
            jnp.zeros((B, H), jnp.float32))
    _, (C_in, n_in, m_in) = lax.scan(step, init, xs)
    C_in = jnp.moveaxis(C_in, 0, 2)
    n_in = jnp.moveaxis(n_in, 0, 2)
    m_in = jnp.moveaxis(m_in, 0, 2)

    lower = jnp.tril(jnp.ones((L, L), dtype=bool))
    D = b[..., :, None] - b[..., None, :] + log_i[..., None, :]
    D = jnp.where(lower, D, -jnp.inf)
    inter = b + m_in[..., None]
    m_t = jnp.maximum(inter, D.max(-1))
    P = jnp.exp(D - m_t[..., None])
    W = jnp.einsum('bhctd,bhcsd->bhcts', q, k) * P
    s_in = jnp.exp(inter - m_t)
    num = (jnp.einsum('bhcts,bhcsv->bhctv', W, v)
           + s_in[..., None] * jnp.einsum('bhcvk,bhctk->bhctv', C_in, q))
    den = W.sum(-1) + s_in * jnp.einsum('bhck,bhctk->bhct', n_in, q)
    h = num / jnp.maximum(jnp.abs(den), jnp.exp(-m_t))[..., None]
    return h.reshape(B, H, S, dv)


def mlstm_branch(z, conv_w, conv_b, b_mgate, norm_g):
    B, S, _ = z.shape
    qk = jax.nn.silu(centred_dwconv(z[..., OFF_MQ:OFF_MV], conv_w, conv_b))
    to_heads = lambda t: t.reshape(B, S, MLSTM_HEADS, MLSTM_DH).transpose(0, 2, 1, 3)
    q = to_heads(qk[..., :MLSTM_W])
    k = to_heads(qk[..., MLSTM_W:])
    v = to_heads(z[..., OFF_MV:OFF_MO])
    o = z[..., OFF_MO:OFF_MG]
    gates = z[..., OFF_MG:OFF_MG + N_MGATES].astype(jnp.float32) + b_mgate.astype(jnp.float32)
    gates = gates.reshape(B, S, 2, 2, MLSTM_HEADS).transpose(2, 3, 0, 4, 1)
    h_fwd = mlstm_chunkwise(q, k, v, gates[0, 0], gates[0, 1])
    flip = lambda t: jnp.flip(t, axis=2)
    h_bwd = flip(mlstm_chunkwise(flip(q), flip(k), flip(v),
                                 jnp.flip(gates[1, 0], -1), jnp.flip(gates[1, 1], -1)))
    h = h_fwd + h_bwd
    mu = h.mean(-1, keepdims=True)
    var = jnp.mean((h - mu) ** 2, axis=-1, keepdims=True)
    h = ((h - mu) * lax.rsqrt(var + EPS)).transpose(0, 2, 1, 3).reshape(B, S, MLSTM_W)
    h = h * norm_g.astype(jnp.float32) * jax.nn.sigmoid(o.astype(jnp.float32))
    return h.astype(z.dtype)


def diff_attention_branch(z, lam, norm_g, lam_init):
    B, S, _ = z.shape
    H = DIFF_HEADS
    q = z[..., OFF_DQ:OFF_DK].reshape(B, S, H, 2, DIFF_DH).transpose(0, 2, 3, 1, 4)
    k = z[..., OFF_DK:OFF_DV].reshape(B, S, H, 2, DIFF_DH).transpose(0, 2, 3, 1, 4)
    v = z[..., OFF_DV:OFF_GATE].reshape(B, S, H, DIFF_DV).transpose(0, 2, 1, 3)
    lamf = lam.astype(jnp.float32)
    lam_full = (jnp.exp(jnp.sum(lamf[0] * lamf[1])) - jnp.exp(jnp.sum(lamf[2] * lamf[3]))
                + lam_init)
    slopes = jnp.exp2(-8.0 * jnp.arange(1, H + 1, dtype=jnp.float32) / H)
    scale = DIFF_DH ** -0.5
    NB = S // Q_BLOCK
    qb = q.reshape(B, H, 2, NB, Q_BLOCK, DIFF_DH).transpose(3, 0, 1, 2, 4, 5)
    pos = jnp.arange(S, dtype=jnp.int32)

    def block(args):
        q_blk, start = args
        s = jnp.einsum('bhiqd,bhikd->bhiqk', q_blk, k).astype(jnp.float32) * scale
        tq = start + jnp.arange(Q_BLOCK, dtype=jnp.int32)
        dist = jnp.abs(tq[:, None] - pos[None, :]).astype(jnp.float32)
        s = s - (slopes[:, None, None] * dist)[None, :, None]
        p = jax.nn.softmax(s, axis=-1)
        a = p[:, :, 0] - lam_full * p[:, :, 1]
        return jnp.einsum('bhqk,bhkv->bhqv', a.astype(v.dtype), v)

    starts = jnp.arange(NB, dtype=jnp.int32) * Q_BLOCK
    o = lax.map(block, (qb, starts))
    o = o.transpose(1, 2, 0, 3, 4).reshape(B, H, S, DIFF_DV).astype(jnp.float32)
    o = o * lax.rsqrt(jnp.mean(o * o, axis=-1, keepdims=True) + EPS)
    o = o * norm_g.astype(jnp.float32) * (1.0 - lam_init)
    return o.transpose(0, 2, 1, 3).reshape(B, S, DIFF_W).astype(z.dtype)


def expert_choice_ffn(h, w_router, w_gate, w_up, w_down):
    B, S, _ = h.shape
    cap = EC_CAPACITY * S // N_EXPERTS
    aff = jax.nn.softmax(jnp.einsum('bsd,de->bse', h, w_router).astype(jnp.float32), axis=-1)
    g, idx = lax.top_k(aff.transpose(0, 2, 1), cap)
    bidx = jnp.arange(B)[:, None, None]
    xe = h[bidx, idx]
    hid = (jax.nn.silu(jnp.einsum('becd,edf->becf', xe, w_gate))
           * jnp.einsum('becd,edf->becf', xe, w_up))
    ye = jnp.einsum('becf,efd->becd', hid, w_down) * g[..., None].astype(h.dtype)
    return jnp.zeros_like(h).at[bidx, idx].add(ye)


def setup_inputs(seed: int = 0) -> dict:
    key = jax.random.key(seed)
    ks = jax.random.split(key, 20)
    nrm = lambda k, shape, s: jax.random.normal(k, shape, jnp.float32) * s
    i_bias = nrm(ks[3], (DEPTH, 2, MLSTM_HEADS), 0.1)
    f_bias = jnp.linspace(3.0, 6.0, MLSTM_HEADS, dtype=jnp.float32) + nrm(ks[4], (DEPTH, 2, MLSTM_HEADS), 0.1)
    b_mgate = jnp.stack([i_bias, f_bias], axis=2).reshape(DEPTH, N_MGATES)
    return {
        "x": nrm(ks[0], (BATCH, SEQ, D_MODEL), 1.0),
        "norm_mix_g": 1.0 + nrm(ks[1], (DEPTH, D_MODEL), 0.01),
        "w_in": nrm(ks[2], (DEPTH, D_MODEL, IN_COLS), D_MODEL ** -0.5),
        "b_mgate": b_mgate,
        "conv_w": nrm(ks[5], (DEPTH, CONV_W, 2 * MLSTM_W), CONV_W ** -0.5),
        "conv_b": nrm(ks[6], (DEPTH, 2 * MLSTM_W), 0.01),
        "mlstm_norm_g": 1.0 + nrm(ks[7], (DEPTH, MLSTM_W), 0.01),
        "diff_lam": nrm(ks[8], (DEPTH, 4, DIFF_DH), 0.1),
        "diff_norm_g": 1.0 + nrm(ks[9], (DEPTH, DIFF_DV), 0.01),
        "w_up_a": nrm(ks[10], (DEPTH, MLSTM_W, D_MODEL), MLSTM_W ** -0.5),
        "w_up_b": nrm(ks[11], (DEPTH, DIFF_W, D_MODEL), DIFF_W ** -0.5),
        "w_out": nrm(ks[12], (DEPTH, D_MODEL, D_MODEL), D_MODEL ** -0.5),
        "norm_ffn_g": 1.0 + nrm(ks[13], (DEPTH, D_MODEL), 0.01),
        "w_router": nrm(ks[14], (DEPTH, D_MODEL, N_EXPERTS), D_MODEL ** -0.5),
        "w_gate_e": nrm(ks[15], (DEPTH, N_EXPERTS, D_MODEL, D_FF_EXPERT), D_MODEL ** -0.5),
        "w_up_e": nrm(ks[16], (DEPTH, N_EXPERTS, D_MODEL, D_FF_EXPERT), D_MODEL ** -0.5),
        "w_down_e": nrm(ks[17], (DEPTH, N_EXPERTS, D_FF_EXPERT, D_MODEL), D_FF_EXPERT ** -0.5),
        "norm_f_g": 1.0 + nrm(ks[18], (D_MODEL,), 0.01),
    }


def reference(x, norm_mix_g, w_in, b_mgate, conv_w, conv_b, mlstm_norm_g, diff_lam, diff_norm_g,
              w_up_a, w_up_b, w_out, norm_ffn_g, w_router, w_gate_e, w_up_e, w_down_e, norm_f_g):
    for l in range(DEPTH):
        lam_init = 0.8 - 0.6 * math.exp(-0.3 * l)
        h = rms_norm(x, norm_mix_g[l])
        z = jnp.einsum('bsd,dc->bsc', h, w_in[l])
        y_a = jnp.einsum('bsw,wd->bsd', mlstm_branch(z, conv_w[l], conv_b[l], b_mgate[l], mlstm_norm_g[l]), w_up_a[l])
        y_b = jnp.einsum('bsw,wd->bsd', diff_attention_branch(z, diff_lam[l], diff_norm_g[l], lam_init), w_up_b[l])
        g_a = jax.nn.sigmoid(z[..., OFF_GATE:OFF_GATE + D_MODEL])
        g_b = jax.nn.sigmoid(z[..., OFF_GATE + D_MODEL:])
        x = x + jnp.einsum('bsd,de->bse', g_a * y_a + g_b * y_b, w_out[l])
        h2 = rms_norm(x, norm_ffn_g[l])
        x = x + expert_choice_ffn(h2, w_router[l], w_gate_e[l], w_up_e[l], w_down_e[l])
    return rms_norm(x, norm_f_g)
```

```python
import math
from contextlib import ExitStack

import numpy as np
import ml_dtypes
import concourse.bass as bass
import concourse.mybir as mybir
from concourse.alu_op_type import AluOpType as ALU
from concourse.bass_utils import run_bass_kernel_spmd

F32, BF16 = mybir.dt.float32, mybir.dt.bfloat16
AF = mybir.ActivationFunctionType
AX = mybir.AxisListType

NCORES = 8
NSEQ = 2
S_LEN = 2048
D = 1024
NT = 16
DEPTH = 2
IN_COLS = 5648
OFF_MG = 2048
OFF_DQ = 2064
OFF_DK = 2576
OFF_DV = 3088
OFF_GATE = 3600
NEXP = 16
CAP = 256
EPS = 1e-6

CF_ID, CF_TL, CF_TU, CF_ONE, CF_IOTA = 0, 128, 256, 384, 512
CF_L0 = 768
LP_BMG, LP_CW, LP_CB, LP_MNG, LP_DNG, LP_LAM, LP_WR = 0, 256, 296, 304, 816, 944, 1200
LP = 1328
CF_BM = CF_L0 + DEPTH * LP
NCF = CF_BM + 128
CB_ID, CB_TL, CB_TU, CB_TS, CB_ONE, CB_CORR = 0, 128, 256, 384, 512, 640
CB_TOK = 640 + 4 * 128
NCB = CB_TOK + 2 * NT


class Buf:
    __slots__ = ("name", "w", "r", "dsem", "dcnt")

    def __init__(self, name):
        self.name = name
        self.w = None
        self.r = []
        self.dsem = None
        self.dcnt = 0


class Tok:
    __slots__ = ("sem", "val", "holder", "eng")

    def __init__(self, sem, val, holder=None, eng=None):
        self.sem, self.val, self.holder, self.eng = sem, val, holder, eng


class Sched:
    def __init__(self, nc):
        self.nc = nc
        self.E = {"pe": nc.tensor, "act": nc.scalar, "dve": nc.vector, "pool": nc.gpsimd, "sp": nc.sync}
        self.sem = {e: nc.alloc_semaphore("sem_" + e) for e in self.E}
        self.cnt = {e: 0 for e in self.E}
        self.known = {e: {} for e in self.E}
        self.dbufs = []
        self.free_dsems_k = {}
        self.dkind = {}
        self.nwait = 0
        self.nins = 0

    def _wait(self, e, toks):
        need = {}
        for t in toks:
            if t is None:
                continue
            v = t.holder.dcnt if t.holder is not None else t.val
            k = id(t.sem)
            if k not in need or need[k][1] < v:
                need[k] = (t.sem, v, t)
        for k, (sem, v, t) in need.items():
            if e == "pe" and t.eng == "pe":
                continue
            if self.known[e].get(k, 0) >= v:
                continue
            self.E[e].wait_ge(sem, v)
            self.known[e][k] = v
            self.nwait += 1

    def _deps(self, reads, writes):
        toks = []
        for b in reads:
            toks.append(b.w)
        for b in writes:
            toks.append(b.w)
            toks.extend(b.r)
        return toks

    def _mark(self, tok, reads, writes):
        for b in reads:
            b.r = [t for t in b.r if t.sem is not tok.sem] + [tok]
        for b in writes:
            b.w = tok
            b.r = []

    def op(self, e, fn, reads=(), writes=()):
        self._wait(e, self._deps(reads, writes))
        ins = fn()
        self.cnt[e] += 1
        ins.then_inc(self.sem[e], 1)
        self.nins += 1
        self._mark(Tok(self.sem[e], self.cnt[e], None, e), reads, writes)

    def dma(self, q, out, in_, holder, reads=(), writes=(), **kw):
        self._wait(q, self._deps(reads, writes))
        self._dsem(q, holder)
        ins = self.E[q].dma_start(out=out, in_=in_, **kw)
        holder.dcnt += 16
        ins.then_inc(holder.dsem, 16)
        self.nins += 1
        self._mark(Tok(holder.dsem, holder.dcnt, holder, "dma"), reads, writes)

    def _dsem(self, q, holder):
        kind = "sw" if q == "pool" else "hw"
        if holder.dsem is None:
            fl = self.free_dsems_k.setdefault(kind, [])
            if fl:
                holder.dsem, holder.dcnt = fl.pop()
            else:
                self.nsem = getattr(self, "nsem", 0) + 1
                holder.dsem = self.nc.alloc_semaphore("dsem_%d" % self.nsem)
            self.dbufs.append(holder)
            self.dkind[id(holder)] = kind
        assert self.dkind[id(holder)] == kind, "buffer mixes SW and HW DMA queues"

    def idma(self, out, in_, holder, out_off=None, in_off=None, reads=(), writes=(), **kw):
        self._wait("pool", self._deps(reads, writes))
        self._dsem("pool", holder)
        ins = self.nc.gpsimd.indirect_dma_start(out=out, out_offset=out_off, in_=in_, in_offset=in_off, **kw)
        holder.dcnt += 16
        ins.then_inc(holder.dsem, 16)
        self.nins += 1
        self._mark(Tok(holder.dsem, holder.dcnt, holder, "dma"), reads, writes)

    def recycle(self, bufs):
        for b in bufs:
            if b.dsem is not None and b in self.dbufs:
                self.dbufs.remove(b)
                self.free_dsems_k[self.dkind[id(b)]].append((b.dsem, b.dcnt))

    def barrier(self):
        for e in self.E:
            toks = [Tok(self.sem[e2], self.cnt[e2], None, "x") for e2 in self.E if self.cnt[e2] > 0]
            toks += [Tok(b.dsem, b.dcnt, b, "dma") for b in self.dbufs]
            self._wait(e, toks)


class Tile:
    def __init__(self, t, nb, name):
        self.t = t
        self.b = [Buf("%s.%d" % (name, i)) for i in range(nb)]
        self.B = self.b[0]


class Scope:
    def __init__(self, K):
        self.K = K
        self.st = ExitStack()
        self.tiles = []

    def sb(self, name, shape, dt, nb=1):
        self.K.uid += 1
        nm = "%s_%d" % (name, self.K.uid)
        t = self.st.enter_context(self.K.nc.sbuf_tensor(nm, list(shape), dt))
        tl = Tile(t, nb, nm)
        self.tiles.append(tl)
        return tl

    def close(self):
        self.K.S.barrier()
        for tl in self.tiles:
            self.K.S.recycle(tl.b)
        self.st.close()


class K:
    def __init__(self, dbg=None, stop=None, nseq=NSEQ):
        self.dbg = dbg or []
        self.stop = stop
        self.nseq = nseq
        self.uid = 0
        self.dumps = {}
        nc = self.nc = bass.Bass("TRN2", target_bir_lowering=False)
        self.S = Sched(nc)
        dt = nc.dram_tensor
        self.x = dt("x", [NSEQ, S_LEN, D], F32, kind="ExternalInput").ap()
        self.w_in = dt("w_in", [DEPTH, D, IN_COLS], F32, kind="ExternalInput").ap()
        self.w_up_a = dt("w_up_a", [DEPTH, 512, D], F32, kind="ExternalInput").ap()
        self.w_up_b = dt("w_up_b", [DEPTH, 512, D], F32, kind="ExternalInput").ap()
        self.w_out = dt("w_out", [DEPTH, D, D], F32, kind="ExternalInput").ap()
        self.use_moe = stop not in ("M0", "M1", "M3", "M4", "Eroute", "Es1", "Es2", "Es3", "Es4", "Es5", "Es6", "Es7", "Esel", "Eidx", "Egather")
        if self.use_moe:
            self.w_gate_e = dt("w_gate_e", [DEPTH, NEXP, D, 2 * D], F32, kind="ExternalInput").ap()
            self.w_up_e = dt("w_up_e", [DEPTH, NEXP, D, 2 * D], F32, kind="ExternalInput").ap()
            self.w_down_e = dt("w_down_e", [DEPTH, NEXP, 2 * D, D], F32, kind="ExternalInput").ap()
        self.cf_d = dt("cf", [128, NCF], F32, kind="ExternalInput").ap()
        self.cb_d = dt("cb", [128, NCB], BF16, kind="ExternalInput").ap()
        self.grows = dt("grows", [5, 128, D], F32, kind="ExternalInput").ap()
        self.kaug = dt("kaug", [2, 32, S_LEN], BF16, kind="ExternalInput").ap()
        self.qaug = dt("qaug", [4, 32, S_LEN], BF16, kind="ExternalInput").ap()
        self.y = dt("y", [NSEQ, S_LEN, D], F32, kind="ExternalOutput").ap()
        self.xres = [dt("xres%d" % i, [S_LEN, D], F32).ap() for i in range(NSEQ)]
        self.h2d = [dt("h2d%d" % i, [S_LEN, D], BF16).ap() for i in range(NSEQ)]
        self.xed = dt("xed", [NEXP, NSEQ, 128, 8, CAP], BF16).ap()
        self.yed = dt("yed", [NSEQ, 2 * NEXP, 128, D], BF16).ap()
        self.xres_b = [[Buf("xres%d_%d" % (s, j)) for j in range(NT)] for s in range(NSEQ)]

    def mm(self, out, lhsT, rhs, start, stop, r=(), w=()):
        nc = self.nc
        self.S.op("pe", lambda: nc.tensor.matmul(out, lhsT=lhsT, rhs=rhs, start=start, stop=stop), r, w)

    def tr(self, out, in_, ident, r=(), w=()):
        nc = self.nc
        self.S.op("pe", lambda: nc.tensor.transpose(out, in_, ident), r, w)

    def act(self, out, in_, func, r=(), w=(), **kw):
        nc = self.nc
        self.S.op("act", lambda: nc.scalar.activation(out=out, in_=in_, func=func, **kw), r, w)

    def tsc(self, out, in0, s1, s2, op0, op1=None, r=(), w=(), eng="dve", accum_out=None):
        E = self.nc.vector if eng == "dve" else self.nc.gpsimd
        kw = {}
        if op1 is not None:
            kw["op1"] = op1
        if accum_out is not None:
            kw["accum_out"] = accum_out
        self.S.op(eng, lambda: E.tensor_scalar(out=out, in0=in0, scalar1=s1, scalar2=s2, op0=op0, **kw), r, w)

    def tt(self, out, in0, in1, op, r=(), w=(), eng="dve"):
        E = self.nc.vector if eng == "dve" else self.nc.gpsimd
        self.S.op(eng, lambda: E.tensor_tensor(out=out, in0=in0, in1=in1, op=op), r, w)

    def stt(self, out, in0, scalar, in1, op0, op1, r=(), w=()):
        nc = self.nc
        self.S.op("dve", lambda: nc.vector.scalar_tensor_tensor(out=out, in0=in0, scalar=scalar, in1=in1,
                                                                 op0=op0, op1=op1), r, w)

    def cp(self, out, in_, r=(), w=(), eng="dve"):
        if eng == "act":
            nc = self.nc
            self.S.op("act", lambda: nc.scalar.copy(out=out, in_=in_), r, w)
        else:
            E = self.nc.vector if eng == "dve" else self.nc.gpsimd
            self.S.op(eng, lambda: E.tensor_copy(out=out, in_=in_), r, w)

    def recip(self, out, in_, r=(), w=()):
        nc = self.nc
        self.S.op("dve", lambda: nc.vector.reciprocal(out=out, in_=in_), r, w)

    def memset(self, ap, val, w=(), eng="dve"):
        E = self.nc.vector if eng == "dve" else self.nc.gpsimd
        self.S.op(eng, lambda: E.memset(ap, val), (), w)

    def dump(self, name, src_ap, shape, dtype, rbufs=()):
        if name not in self.dbg:
            return
        self.S.barrier()
        d = self.nc.dram_tensor("dbg_" + name, list(shape), dtype, kind="ExternalOutput").ap()
        hb = Buf("dbg_" + name)
        self.S.dma("sp", d, src_ap, hb, reads=rbufs)
        self.S.barrier()
        self.S.recycle([hb])
        self.dumps[name] = "dbg_" + name

    def build(self):
        nc, S = self.nc, self.S
        g = Scope(self)
        self.g = g
        self.cf = g.sb("cf", [128, NCF], F32)
        self.cb = g.sb("cb", [128, NCB], BF16)
        S.dma("sp", self.cf.t[:], self.cf_d, self.cf.B, writes=[self.cf.B])
        S.dma("sp", self.cb.t[:], self.cb_d, self.cb.B, writes=[self.cb.B])
        self.ps = []
        self.pb = []
        for i in range(8):
            self.ps.append(nc.alloc_psum_tensor("ps%d" % i, [128, 512], F32))
            self.pb.append(Buf("ps%d" % i))
        self.psb = [self.ps[6].bitcast(BF16), self.ps[7].bitcast(BF16)]
        self.pbb = [self.pb[6], self.pb[7]]
        self.aff2 = g.sb("aff2", [128, NT, 32], F32)
        self.memset(self.aff2.t[:], 0.0, w=[self.aff2.B])
        self.h2d_b = [[Buf("h2d") for j in range(NT)] for s in range(NSEQ)]
        self.xed_b = [[Buf("xed") for s in range(NSEQ)] for e in range(NEXP)]
        self.yed_b = [[Buf("yed") for i in range(2 * NEXP)] for s in range(NSEQ)]
        S.barrier()
        try:
            for l in range(DEPTH):
                for s in range(self.nseq):
                    self.phase_M(l, s)
                self.phase_E(l)
        except StopIteration:
            pass
        S.barrier()
        return nc

    def CF(self, c0, n):
        return self.cf.t[:, c0:c0 + n]

    def CB(self, c0, n):
        return self.cb.t[:, c0:c0 + n]

    def check_stop(self, name):
        if self.stop == name:
            raise StopIteration

    def rms_stats(self, xin_ap, xin_b, junk_ap, junk_b, ss_ap, sq_ap, rstd_ap, sb_, n=D):
        self.act(junk_ap, xin_ap, AF.Square, r=[xin_b], w=[junk_b, sb_], accum_out=ss_ap)
        self.act(sq_ap, ss_ap, AF.Sqrt, r=[sb_, self.epsc_b], w=[sb_], bias=self.epsc, scale=1.0 / n)
        self.recip(rstd_ap, sq_ap, r=[sb_], w=[sb_])

    def phase_M(self, l, s):
        nc, S = self.nc, self.S
        ps, pb, psb, pbb = self.ps, self.pb, self.psb, self.pbb
        lam_init = 0.8 - 0.6 * math.exp(-0.3 * l)
        lb = CF_L0 + l * LP
        M = Scope(self)
        hT = M.sb("hT", [128, 8, S_LEN], BF16)
        hmT = M.sb("hmT", [128, 4, S_LEN], BF16)
        odT = M.sb("odT", [128, 4, S_LEN], BF16)
        epsc = M.sb("epsc", [128, 1], F32)
        self.memset(epsc.t[:], EPS, w=[epsc.B])
        self.epsc = epsc.t[:, 0:1]
        self.epsc_b = epsc.B

        def xsrc(j):
            if l == 0:
                return self.x[s, j * 128:(j + 1) * 128, :], None
            return self.xres[s][j * 128:(j + 1) * 128, :], self.xres_b[s][j]

        sc = Scope(self)
        grow = sc.sb("grow", [128, D], F32)
        S.dma("sp", grow.t[:], self.grows[l], grow.B, writes=[grow.B])
        xin = sc.sb("xin", [128, 4, D], F32, nb=4)
        xn = sc.sb("xn", [128, 4, D], F32, nb=4)
        st = sc.sb("st", [128, NT, 4], F32, nb=NT)
        def m0_a(j):
            b = j % 4
            src, sbuf = xsrc(j)
            S.dma("sp", xin.t[:, b, :], src, xin.b[b], reads=[sbuf] if sbuf else [], writes=[xin.b[b]])
            self.rms_stats(xin.t[:, b, :], xin.b[b], xn.t[:, b, :], xn.b[b], st.t[:, j, 0:1], st.t[:, j, 1:2],
                           st.t[:, j, 2:3], st.b[j])
            self.stt(xn.t[:, b, :], xin.t[:, b, :], st.t[:, j, 2:3], grow.t[:], ALU.mult, ALU.mult,
                     r=[xin.b[b], st.b[j], grow.B], w=[xn.b[b]])

        def m0_b(j):
            b = j % 4
            pa, pc = (j % 2) * 2, (j % 2) * 2 + 1
            for kc in range(8):
                bk = pa if kc < 4 else pc
                self.tr(ps[bk][:, (kc % 4) * 128:(kc % 4 + 1) * 128], xn.t[:, b, kc * 128:(kc + 1) * 128],
                        self.CF(CF_ID, 128), r=[xn.b[b], self.cf.B], w=[pb[bk]])
            self.cp(hT.t[:, 0:4, j * 128:(j + 1) * 128], ps[pa][:, :].rearrange("p (k t) -> p k t", k=4),
                    r=[pb[pa]], w=[hT.B], eng="act")
            self.cp(hT.t[:, 4:8, j * 128:(j + 1) * 128], ps[pc][:, :].rearrange("p (k t) -> p k t", k=4),
                    r=[pb[pc]], w=[hT.B], eng="dve")

        for j in range(NT + 2):
            if j < NT:
                m0_a(j)
            if j >= 2:
                m0_b(j - 2)
        sc.close()
        self.dump("hT", hT.t[:], [128, 8, S_LEN], BF16)
        self.check_stop("M0")

        sc = Scope(self)
        wgt = sc.sb("wgt", [128, 8, 16], BF16)
        win_l = self.w_in[l].rearrange("(kc p) c -> p kc c", p=128)
        S.dma("pool", wgt.t[:], win_l[:, :, OFF_MG:OFF_MG + 16], wgt.B, writes=[wgt.B])
        for j in range(NT):
            for kc in range(8):
                self.mm(ps[0][:, j * 16:(j + 1) * 16], hT.t[:, kc, j * 128:(j + 1) * 128], wgt.t[:, kc, :],
                        kc == 0, kc == 7, r=[wgt.B], w=[pb[0]])
        GT = sc.sb("GT", [128, 256], F32)
        self.tt(GT.t[:], ps[0][:, 0:256], self.CF(lb + LP_BMG, 256), ALU.add, r=[pb[0], self.cf.B], w=[GT.B])
        GT5 = GT.t[:].rearrange("p (j a b h) -> p j a b h", j=NT, a=2, b=2, h=4)
        LI = sc.sb("LI", [128, 2, NT, 4], F32)
        LF = sc.sb("LF", [128, 2, NT, 4], F32)
        tmpg = sc.sb("tmpg", [128, 2, NT, 4], F32)
        v4 = lambda t: t.t[:].rearrange("p a j h -> p j a h")
        self.cp(v4(LI), GT5[:, :, :, 0, :], r=[GT.B], w=[LI.B])
        self.act(v4(tmpg), GT5[:, :, :, 1, :], AF.Exp, r=[GT.B], w=[tmpg.B], scale=-1.0)
        self.act(tmpg.t[:], tmpg.t[:], AF.Ln, r=[tmpg.B], w=[tmpg.B], bias=1.0)
        self.tsc(LF.t[:], tmpg.t[:], -1.0, None, ALU.mult, r=[tmpg.B], w=[LF.B])
        fl = lambda ap: ap.rearrange("p j h -> p (j h)")
        self.mm(ps[1][:, 0:64], self.CF(CF_TL, 128), fl(LF.t[:, 0, :, :]), True, True, r=[LF.B, self.cf.B], w=[pb[1]])
        self.mm(ps[1][:, 64:128], self.CF(CF_TU, 128), fl(LF.t[:, 1, :, :]), True, True, r=[LF.B, self.cf.B], w=[pb[1]])
        self.mm(ps[2][:, 0:128], self.CF(CF_ONE, 128), LF.t[:].rearrange("p a j h -> p (a j h)"), True, True,
                r=[LF.B, self.cf.B], w=[pb[2]])
        A = sc.sb("A", [128, 2, NT, 4], F32)
        EB = sc.sb("EB", [128, 2, NT, 4], F32)
        EG = sc.sb("EG", [128, 2, NT, 4], F32)
        f3 = lambda t: t.t[:].rearrange("p a j h -> p (a j h)")
        self.tt(f3(tmpg), f3(LI), ps[1][:, 0:128], ALU.subtract, r=[LI.B, pb[1]], w=[tmpg.B])
        lnk = sc.sb("lnk", [128, 1], F32)
        self.memset(lnk.t[:], math.log(128 ** -0.5), w=[lnk.B])
        self.act(A.t[:], tmpg.t[:], AF.Exp, r=[tmpg.B, lnk.B], w=[A.B], bias=lnk.t[:, 0:1])
        self.act(f3(EB), ps[1][:, 0:128], AF.Exp, r=[pb[1]], w=[EB.B])
        self.act(f3(EG), ps[2][:, 0:128], AF.Exp, r=[pb[2]], w=[EG.B])
        S.barrier()

        wh = sc.sb("wh", [128, 2, 8, 4, 128], BF16, nb=2)
        pre = sc.sb("pre", [128, 2, S_LEN + 4], BF16, nb=2)
        dg = sc.sb("dg", [128, 2, 5, 128], BF16, nb=2)
        qk = sc.sb("qk", [128, 2, S_LEN], BF16, nb=2)
        ktok = sc.sb("ktok", [128, NT, 128], BF16)
        V = [sc.sb("Vf", [128, NT, 129], BF16), sc.sb("Vb", [128, NT, 129], BF16)]
        sigo = sc.sb("sigo", [128, NT, 128], BF16)
        WT = [sc.sb("WTf", [128, NT, 128], BF16, nb=NT), sc.sb("WTb", [128, NT, 128], BF16, nb=NT)]
        Hs = [sc.sb("Hsf", [128, NT, 129], F32, nb=NT), sc.sb("Hsb", [128, NT, 129], F32, nb=NT)]
        Sf = [sc.sb("Sf", [128, 129], F32), sc.sb("Sb", [128, 129], F32)]
        tmpS = [sc.sb("tSf", [128, 129], F32), sc.sb("tSb", [128, 129], F32)]
        Sbf = [sc.sb("Sfb", [128, 129], BF16), sc.sb("Sbb", [128, 129], BF16)]
        den = sc.sb("den", [128, 2, NT, 2], F32)
        hs = sc.sb("hs", [128, NT, 128], F32, nb=NT)
        bst = sc.sb("bst", [128, NT, 6], F32, nb=NT)
        mv = sc.sb("mv", [128, NT, 4], F32)
        xh = sc.sb("xh", [128, 4, 128], F32, nb=4)
        hmt = sc.sb("hmt", [128, 4, 128], BF16, nb=4)
        nmr = sc.sb("nmr", [128, NT, 1], F32)
        self.memset(pre.t[:], 0.0, w=[pre.b[0], pre.b[1]])

        def load_wh(h_):
            for qi in range(4):
                c0 = qi * 512 + h_ * 128
                S.dma("pool", wh.t[:, h_ % 2, :, qi, :], win_l[:, :, c0:c0 + 128], wh.b[h_ % 2], writes=[wh.b[h_ % 2]])

        load_wh(0)
        for hd in range(4):
            for qi in range(2):
                cwb = lb + LP_CW + (qi * 4 + hd) * 5
                for tp in range(5):
                    self.tsc(dg.t[:, qi, tp, :], self.CB(CB_ID, 128), self.CF(cwb + tp, 1), None, ALU.mult,
                             r=[self.cb.B, self.cf.B], w=[dg.b[qi]])
                for n in range(4):
                    bk = qi * 4 + n
                    for kc in range(8):
                        self.mm(ps[bk][:, :], wh.t[:, hd % 2, kc, qi, :], hT.t[:, kc, n * 512:(n + 1) * 512], kc == 0, kc == 7,
                                r=[wh.b[hd % 2]], w=[pb[bk]])
                    self.cp(pre.t[:, qi, 2 + n * 512:2 + (n + 1) * 512], ps[bk][:, :], r=[pb[bk]], w=[pre.b[qi]], eng="act")
            for qi in range(2):
                for n in range(4):
                    bk = qi * 4 + n
                    for tp in range(5):
                        self.mm(ps[bk][:, :], dg.t[:, qi, tp, :], pre.t[:, qi, tp + n * 512:tp + (n + 1) * 512], tp == 0,
                                tp == 4, r=[dg.b[qi], pre.b[qi]], w=[pb[bk]])
                    self.act(qk.t[:, qi, n * 512:(n + 1) * 512], ps[bk][:, :], AF.Silu, r=[pb[bk], self.cf.B],
                             w=[qk.b[qi]], bias=self.CF(lb + LP_CB + qi * 4 + hd, 1))
            for j in range(NT):
                bk = j % 4
                for kc in range(8):
                    self.mm(ps[bk][:, 0:256], hT.t[:, kc, j * 128:(j + 1) * 128],
                            wh.t[:, hd % 2, kc, 2:4, :], kc == 0, kc == 7, r=[wh.b[hd % 2]], w=[pb[bk]])
                for dr in range(2):
                    self.act(V[dr].t[:, j, 0:128], ps[bk][:, 0:128], AF.Copy, r=[pb[bk], A.B], w=[V[dr].B],
                             scale=A.t[:, dr, j, hd:hd + 1])
                self.act(sigo.t[:, j, :], ps[bk][:, 128:256], AF.Sigmoid, r=[pb[bk]], w=[sigo.B])
            for dr in range(2):
                self.cp(V[dr].t[:, :, 128:129], A.t[:, dr, :, hd:hd + 1], r=[A.B], w=[V[dr].B])
            if hd + 1 < 4:
                load_wh(hd + 1)
            for j in range(NT):
                g8 = j // 8
                self.tr(psb[g8][:, (j % 8) * 128:(j % 8 + 1) * 128], qk.t[:, 1, j * 128:(j + 1) * 128],
                        self.CB(CB_ID, 128), r=[qk.b[1], self.cb.B], w=[pbb[g8]])
                if j % 8 == 7:
                    self.cp(ktok.t[:, g8 * 8:(g8 + 1) * 8, :].rearrange("p j d -> p (j d)"), psb[g8][:, :],
                            r=[pbb[g8]], w=[ktok.B], eng="act")
            for c in range(NT):
                bk = c % 2
                cs = slice(c * 128, (c + 1) * 128)
                self.mm(ps[bk][:, 0:128], qk.t[:, 1, cs], qk.t[:, 0, cs], True, True, r=[qk.b[0], qk.b[1]], w=[pb[bk]])
                self.tt(WT[0].t[:, c, :], ps[bk][:, 0:128], self.CB(CB_TL, 128), ALU.mult, r=[pb[bk], self.cb.B],
                        w=[WT[0].b[c]])
                self.tt(WT[1].t[:, c, :], ps[bk][:, 0:128], self.CB(CB_TU, 128), ALU.mult, r=[pb[bk], self.cb.B],
                        w=[WT[1].b[c]])
            for step in range(NT):
                for dr in range(2):
                    c = step if dr == 0 else NT - 1 - step
                    cp_ = c - 1 if dr == 0 else c + 1
                    cs = slice(c * 128, (c + 1) * 128)
                    hb = dr * 2 + step % 2
                    kb = 4 + dr * 2 + step % 2
                    gcol = dr * 4 + hd
                    if step > 0:
                        self.act(Sbf[dr].t[:], Sf[dr].t[:], AF.Copy, r=[Sf[dr].B, EG.B], w=[Sbf[dr].B],
                                 scale=EG.t[:, dr, cp_, hd:hd + 1])
                    self.mm(ps[hb][:, 0:129], WT[dr].t[:, c, :], V[dr].t[:, c, :], True, step == 0,
                            r=[WT[dr].b[c], V[dr].B], w=[pb[hb]])
                    if step > 0:
                        self.mm(ps[hb][:, 0:129], qk.t[:, 0, cs], Sbf[dr].t[:], False, True,
                                r=[qk.b[0], Sbf[dr].B], w=[pb[hb]])
                    self.tsc(Hs[dr].t[:, c, :], ps[hb][:, 0:129], EB.t[:, dr, c, hd:hd + 1], None, ALU.mult,
                             r=[pb[hb], EB.B], w=[Hs[dr].b[c]])
                    if step < NT - 1:
                        self.mm(ps[kb][:, 0:129], ktok.t[:, c, :], V[dr].t[:, c, :], True, True,
                                r=[ktok.B, V[dr].B], w=[pb[kb]])
                        if step == 0:
                            self.cp(Sf[dr].t[:], ps[kb][:, 0:129], r=[pb[kb]], w=[Sf[dr].B], eng="dve")
                        else:
                            self.stt(Sf[dr].t[:], Sf[dr].t[:], EG.t[:, dr, cp_, hd:hd + 1], ps[kb][:, 0:129],
                                     ALU.mult, ALU.add, r=[Sf[dr].B, EG.B, pb[kb]], w=[Sf[dr].B])
            for dr in range(2):
                self.stt(den.t[:, dr, :, 0:1], Hs[dr].t[:, :, 128:129], -1.0, Hs[dr].t[:, :, 128:129], ALU.mult, ALU.max,
                         r=Hs[dr].b, w=[den.B])
                self.tsc(den.t[:, dr, :, 0:1], den.t[:, dr, :, 0:1], 1.0, None, ALU.max, r=[den.B], w=[den.B])
                self.recip(den.t[:, dr, :, 1:2], den.t[:, dr, :, 0:1], r=[den.B], w=[den.B])
            nc_ = self.nc
            for j in range(NT):
                self.act(hs.t[:, j, :], Hs[0].t[:, j, 0:128], AF.Copy, r=[Hs[0].b[j], den.B], w=[hs.b[j]],
                         scale=den.t[:, 0, j, 1:2])
            for j in range(NT):
                self.stt(hs.t[:, j, :], Hs[1].t[:, j, 0:128], den.t[:, 1, j, 1:2], hs.t[:, j, :], ALU.mult, ALU.add,
                         r=[Hs[1].b[j], den.B, hs.b[j]], w=[hs.b[j]])
            for j in range(NT):
                self.S.op("dve", lambda: nc_.vector.bn_stats(out=bst.t[:, j, :], in_=hs.t[:, j, :]), [hs.b[j]], [bst.b[j]])
            for j in range(NT):
                self.S.op("dve", lambda: nc_.vector.bn_aggr(out=mv.t[:, j, 0:2], in_=bst.t[:, j, :]), [bst.b[j]], [mv.B])
            self.act(mv.t[:, :, 2:3], mv.t[:, :, 1:2], AF.Sqrt, r=[mv.B, self.epsc_b], w=[mv.B], bias=self.epsc, scale=1.0)
            self.recip(mv.t[:, :, 3:4], mv.t[:, :, 2:3], r=[mv.B], w=[mv.B])
            self.stt(nmr.t[:], mv.t[:, :, 0:1], -1.0, mv.t[:, :, 3:4], ALU.mult, ALU.mult, r=[mv.B], w=[nmr.B])
            for j in range(NT):
                b = j % 4
                g8 = j // 8
                self.act(xh.t[:, b, :], hs.t[:, j, :], AF.Identity, r=[hs.b[j], mv.B, nmr.B], w=[xh.b[b]],
                         scale=mv.t[:, j, 3:4], bias=nmr.t[:, j, :])
                self.tt(xh.t[:, b, :], xh.t[:, b, :], self.CF(lb + LP_MNG + hd * 128, 128), ALU.mult,
                        r=[xh.b[b], self.cf.B], w=[xh.b[b]], eng="pool")
                self.tt(hmt.t[:, b, :], xh.t[:, b, :], sigo.t[:, j, :], ALU.mult, r=[xh.b[b], sigo.B], w=[hmt.b[b]],
                        eng="pool")
                self.tr(psb[g8][:, (j % 8) * 128:(j % 8 + 1) * 128], hmt.t[:, b, :], self.CB(CB_ID, 128),
                        r=[hmt.b[b], self.cb.B], w=[pbb[g8]])
                if j % 8 == 7:
                    self.cp(hmT.t[:, hd, g8 * 1024:(g8 + 1) * 1024], psb[g8][:, :], r=[pbb[g8]], w=[hmT.B], eng="dve")
        sc.close()
        self.dump("hmT", hmT.t[:], [128, 4, S_LEN], BF16)
        self.check_stop("M1")

        sc = Scope(self)
        wd = sc.sb("wd", [128, 2, 8, 384], BF16, nb=2)
        qA = sc.sb("qA", [128, 2, S_LEN], BF16, nb=2)
        kA = sc.sb("kA", [128, 2, 2, S_LEN], BF16)
        Vd = sc.sb("Vd", [128, NT, 129], BF16)
        PT = sc.sb("PT", [128, 3, 512], BF16, nb=3)
        lm = sc.sb("lm", [128, 136], F32)
        glr = sc.sb("glr", [128, 128], F32)
        rr = sc.sb("rr", [128, 2, 8], F32, nb=2)
        t1 = sc.sb("t1", [128, 2, 128], F32, nb=2)
        ob = sc.sb("ob", [128, 2, 128], F32, nb=2)
        jk = sc.sb("jk", [128, 2, 128], F32, nb=2)
        odt = sc.sb("odt", [128, 2, 128], BF16, nb=2)
        self.memset(kA.t[32:64, :, 1, :], 0.0, w=[kA.B])
        self.memset(qA.t[32:64, 1, :], 0.0, w=[qA.b[1]])
        for v in range(2):
            S.dma("sp", kA.t[64:96, v, 0, :], self.kaug[v], kA.B, writes=[kA.B])
            S.dma("sp", kA.t[0:32, v, 1, :], self.kaug[v], kA.B, writes=[kA.B])
        self.memset(Vd.t[:, :, 128:129], 1.0, w=[Vd.B])
        lamc = lb + LP_LAM
        nc_ = self.nc
        for i2 in range(2):
            self.tt(lm.t[:, 0:64], self.CF(lamc + i2 * 128, 64), self.CF(lamc + i2 * 128 + 64, 64), ALU.mult,
                    r=[self.cf.B, lm.B], w=[lm.B])
            self.S.op("dve", lambda: nc_.vector.reduce_sum(out=lm.t[:, 64 + i2:65 + i2], in_=lm.t[:, 0:64], axis=AX.X),
                      [lm.B], [lm.B])
        self.act(lm.t[:, 66:68], lm.t[:, 64:66], AF.Exp, r=[lm.B], w=[lm.B])
        self.tt(lm.t[:, 68:69], lm.t[:, 66:67], lm.t[:, 67:68], ALU.subtract, r=[lm.B], w=[lm.B])
        self.tsc(lm.t[:, 69:70], lm.t[:, 68:69], float(lam_init), None, ALU.add, r=[lm.B], w=[lm.B])
        lamf = lm.t[:, 69:70]
        self.tsc(glr.t[:], self.CF(lb + LP_DNG, 128), float(1.0 - lam_init), None, ALU.mult, r=[self.cf.B], w=[glr.B])
        def load_wd(h_):
            for i3, off in enumerate((OFF_DQ, OFF_DK, OFF_DV)):
                c0 = off + h_ * 128
                S.dma("pool", wd.t[:, h_ % 2, :, i3 * 128:(i3 + 1) * 128], win_l[:, :, c0:c0 + 128], wd.b[h_ % 2],
                      writes=[wd.b[h_ % 2]])

        load_wd(0)
        for hd in range(4):
            S.dma("sp", qA.t[64:96, 0, :], self.qaug[hd], qA.b[0], writes=[qA.b[0]])
            S.dma("sp", qA.t[0:32, 1, :], self.qaug[hd], qA.b[1], writes=[qA.b[1]])
            for n in range(4):
                ns = slice(n * 512, (n + 1) * 512)
                bk = n % 2
                for kc in range(8):
                    self.mm(ps[bk][:, :], wd.t[:, hd % 2, kc, 0:128], hT.t[:, kc, ns], kc == 0, kc == 7,
                            r=[wd.b[hd % 2]], w=[pb[bk]])
                self.act(qA.t[0:64, 0, ns], ps[bk][0:64, :], AF.Copy, r=[pb[bk]], w=[qA.b[0]], scale=0.125)
                self.act(qA.t[64:128, 1, ns], ps[bk][64:128, :], AF.Copy, r=[pb[bk]], w=[qA.b[1]], scale=0.125)
                bk = 2 + n % 2
                for kc in range(8):
                    self.mm(ps[bk][:, :], wd.t[:, hd % 2, kc, 128:256], hT.t[:, kc, ns], kc == 0, kc == 7,
                            r=[wd.b[hd % 2]], w=[pb[bk]])
                self.cp(kA.t[0:64, 0, 0, ns], ps[bk][0:64, :], r=[pb[bk]], w=[kA.B], eng="act")
                self.cp(kA.t[0:64, 1, 0, ns], ps[bk][0:64, :], r=[pb[bk]], w=[kA.B], eng="dve")
                self.cp(kA.t[64:128, 0, 1, ns], ps[bk][64:128, :], r=[pb[bk]], w=[kA.B], eng="dve")
                self.cp(kA.t[64:128, 1, 1, ns], ps[bk][64:128, :], r=[pb[bk]], w=[kA.B], eng="act")
            for j in range(NT):
                bk = 4 + j % 2
                for kc in range(8):
                    self.mm(ps[bk][:, 0:128], hT.t[:, kc, j * 128:(j + 1) * 128], wd.t[:, hd % 2, kc, 256:384],
                            kc == 0, kc == 7, r=[wd.b[hd % 2]], w=[pb[bk]])
                self.cp(Vd.t[:, j, 0:128], ps[bk][:, 0:128], r=[pb[bk]], w=[Vd.B], eng="act")
            if hd + 1 < 4:
                load_wd(hd + 1)
            slope = 2.0 ** (-2.0 * (hd + 1))
            wmax = int(math.ceil(96.0 / slope / 128.0)) + 1
            iters = [(qt, kb) for qt in range(8) for kb in range(NT)
                     if min(abs(2 * qt + i - kb) for i in range(2)) < wmax]
            kept = {qt: [kb for (q_, kb) in iters if q_ == qt] for qt in range(8)}

            qkb = [0, 1, 6]

            def emit_qk(it):
                qt, kb = iters[it]
                bank = qkb[it % 3]
                ks = slice(kb * 128, (kb + 1) * 128)
                for half in range(2):
                    rel = ["a" if (2 * qt + i) > kb else ("b" if (2 * qt + i) < kb else "d") for i in range(2)]
                    kr = 96 if half == 0 else 128
                    if rel[0] == rel[1] and rel[0] != "d":
                        var = 0 if rel[0] == "a" else 1
                        self.mm(ps[bank][:, half * 256:(half + 1) * 256], kA.t[0:kr, var, half, ks],
                                qA.t[0:kr, half, qt * 256:(qt + 1) * 256], True, True, r=[kA.B, qA.b[half]],
                                w=[pb[bank]])
                    else:
                        for i in range(2):
                            var = 1 if rel[i] == "b" else 0
                            o_ = ps[bank][:, half * 256 + i * 128:half * 256 + (i + 1) * 128]
                            qs = slice((2 * qt + i) * 128, (2 * qt + i + 1) * 128)
                            self.mm(o_, kA.t[0:kr, var, half, ks], qA.t[0:kr, half, qs], True, rel[i] != "d",
                                    r=[kA.B, qA.b[half]], w=[pb[bank]])
                            if rel[i] == "d":
                                self.mm(o_, self.CB(CB_ID, 128), self.CB(CB_CORR + hd * 128, 128), False, True,
                                        w=[pb[bank]])
                self.act(PT.t[:, it % 3, :], ps[bank][:, :], AF.Exp, r=[pb[bank]], w=[PT.b[it % 3]])

            def emit_pv(it):
                qt, kb = iters[it]
                pbuf = it % 3
                for half in range(2):
                    for i in range(2):
                        pv = 2 + half * 2 + i
                        self.mm(ps[pv][:, 0:129], PT.t[:, pbuf, half * 256 + i * 128:half * 256 + (i + 1) * 128],
                                Vd.t[:, kb, :], kb == kept[qt][0], kb == kept[qt][-1], r=[PT.b[pbuf], Vd.B], w=[pb[pv]])
                if kb != kept[qt][-1]:
                    return
                for i in range(2):
                    qb = 2 * qt + i
                    b = qb % 2
                    g8 = qb // 8
                    p0, p1 = ps[2 + i], ps[4 + i]
                    self.recip(rr.t[:, b, 0:1], p0[:, 128:129], r=[pb[2 + i]], w=[rr.b[b]])
                    self.recip(rr.t[:, b, 1:2], p1[:, 128:129], r=[pb[4 + i]], w=[rr.b[b]])
                    self.tt(rr.t[:, b, 2:3], rr.t[:, b, 1:2], lamf, ALU.mult, r=[rr.b[b], lm.B], w=[rr.b[b]])
                    self.tsc(t1.t[:, b, :], p1[:, 0:128], rr.t[:, b, 2:3], None, ALU.mult, r=[pb[4 + i], rr.b[b]],
                             w=[t1.b[b]])
                    self.stt(ob.t[:, b, :], p0[:, 0:128], rr.t[:, b, 0:1], t1.t[:, b, :], ALU.mult, ALU.subtract,
                             r=[pb[2 + i], rr.b[b], t1.b[b]], w=[ob.b[b]])
                    nc_ = self.nc
                    self.S.op("dve", lambda: nc_.vector.scalar_tensor_tensor(
                        out=jk.t[:, b, :], in0=ob.t[:, b, :], scalar=1.0, in1=ob.t[:, b, :],
                        op0=ALU.mult, op1=ALU.mult, accum_out=rr.t[:, b, 3:4]), [ob.b[b]], [jk.b[b], rr.b[b]])
                    self.act(rr.t[:, b, 4:5], rr.t[:, b, 3:4], AF.Ln, r=[rr.b[b], self.epsc_b], w=[rr.b[b]], bias=self.epsc,
                             scale=1.0 / 128)
                    self.act(rr.t[:, b, 5:6], rr.t[:, b, 4:5], AF.Exp, r=[rr.b[b]], w=[rr.b[b]], scale=-0.5)
                    self.stt(odt.t[:, b, :], ob.t[:, b, :], rr.t[:, b, 5:6], glr.t[:], ALU.mult, ALU.mult,
                             r=[ob.b[b], rr.b[b], glr.B], w=[odt.b[b]])
                    self.tr(psb[1][:, (qb % 8) * 128:(qb % 8 + 1) * 128], odt.t[:, b, :], self.CB(CB_ID, 128),
                            r=[odt.b[b], self.cb.B], w=[pbb[1]])
                    if qb % 8 == 7:
                        self.cp(odT.t[:, hd, g8 * 1024:(g8 + 1) * 1024], psb[1][:, :], r=[pbb[1]], w=[odT.B],
                                eng="dve")

            LA = 2
            for n_ in range(len(iters) + LA):
                if n_ < len(iters):
                    emit_qk(n_)
                if n_ >= LA:
                    emit_pv(n_ - LA)
        sc.close()
        self.dump("odT", odT.t[:], [128, 4, S_LEN], BF16)
        self.check_stop("M3")

        sc = Scope(self)
        uT = sc.sb("uT", [128, 8, S_LEN], BF16)
        wga = sc.sb("wga", [128, 2, 2, 8, 128], BF16, nb=2)
        wua = sc.sb("wua", [128, 2, 2, 4, 128], BF16, nb=2)
        sg = sc.sb("sg", [128, 2, 512], F32, nb=2)
        tu = sc.sb("tu", [128, 2, 512], F32, nb=2)
        wupa = self.w_up_a[l].rearrange("(kc p) c -> p kc c", p=128)
        wupb = self.w_up_b[l].rearrange("(kc p) c -> p kc c", p=128)
        for ec in range(8):
            es = slice(ec * 128, (ec + 1) * 128)
            for ab in range(2):
                c0 = OFF_GATE + ab * D + ec * 128
                S.dma("pool", wga.t[:, ec % 2, ab, :, :], win_l[:, :, c0:c0 + 128], wga.b[ec % 2], writes=[wga.b[ec % 2]])
                S.dma("pool", wua.t[:, ec % 2, ab, :, :], (wupa if ab == 0 else wupb)[:, :, es], wua.b[ec % 2],
                      writes=[wua.b[ec % 2]])
            for n in range(4):
                ns = slice(n * 512, (n + 1) * 512)
                b4 = (n % 2) * 4
                for ab in range(2):
                    src = hmT if ab == 0 else odT
                    for kc in range(4):
                        self.mm(ps[b4 + ab][:, :], wua.t[:, ec % 2, ab, kc, :], src.t[:, kc, ns], kc == 0, kc == 3,
                                r=[wua.b[ec % 2]], w=[pb[b4 + ab]])
                    for kc in range(8):
                        self.mm(ps[b4 + 2 + ab][:, :], wga.t[:, ec % 2, ab, kc, :], hT.t[:, kc, ns], kc == 0, kc == 7,
                                r=[wga.b[ec % 2]], w=[pb[b4 + 2 + ab]])
                for ab in range(2):
                    self.act(sg.t[:, ab, :], ps[b4 + 2 + ab][:, :], AF.Sigmoid, r=[pb[b4 + 2 + ab]], w=[sg.b[ab]])
                    self.tt(tu.t[:, ab, :], ps[b4 + ab][:, :], sg.t[:, ab, :], ALU.mult, r=[pb[b4 + ab], sg.b[ab]],
                            w=[tu.b[ab]])
                self.tt(uT.t[:, ec, ns], tu.t[:, 0, :], tu.t[:, 1, :], ALU.add, r=[tu.b[0], tu.b[1]], w=[uT.B])
        S.barrier()
        self.dump("uT", uT.t[:], [128, 8, S_LEN], BF16)
        wo = sc.sb("wo", [128, 8, D], BF16)
        wout_l = self.w_out[l].rearrange("(kc p) c -> p kc c", p=128)
        for half in range(2):
            S.dma("pool", wo.t[:, :, half * 512:(half + 1) * 512], wout_l[:, :, half * 512:(half + 1) * 512], wo.B,
                  writes=[wo.B])
        xin = sc.sb("xin2", [128, 2, D], F32, nb=2)
        xo = sc.sb("xo", [128, 2, D], F32, nb=2)
        grow2 = sc.sb("grow2", [128, D], F32)
        S.dma("sp", grow2.t[:], self.grows[2 + l], grow2.B, writes=[grow2.B])
        xn = sc.sb("xn2", [128, 2, D], F32, nb=2)
        h2b = sc.sb("h2b", [128, 2, D], BF16, nb=2)
        h2T = sc.sb("h2T", [128, 2, 8, 128], F32, nb=2)
        st = sc.sb("st2", [128, NT, 4], F32, nb=NT)
        sm = sc.sb("sm", [128, NT, 4], F32, nb=NT)
        lg = sc.sb("lg", [128, 2, 16], F32, nb=2)
        aff2 = self.aff2
        def p1(j):
            b = j % 2
            src, sbuf = xsrc(j)
            S.dma("sp", xin.t[:, b, :], src, xin.b[b], reads=[sbuf] if sbuf else [], writes=[xin.b[b]])
            for half in range(2):
                bk = (j % 2) * 2 + half
                hs_ = slice(half * 512, (half + 1) * 512)
                for ec in range(8):
                    self.mm(ps[bk][:, :], uT.t[:, ec, j * 128:(j + 1) * 128], wo.t[:, ec, hs_], ec == 0, ec == 7,
                            r=[wo.B], w=[pb[bk]])
                self.tt(xo.t[:, b, hs_], ps[bk][:, :], xin.t[:, b, hs_], ALU.add, r=[pb[bk], xin.b[b]], w=[xo.b[b]])
            S.dma("sp", self.xres[s][j * 128:(j + 1) * 128, :], xo.t[:, b, :], xo.b[b], reads=[xo.b[b]],
                  writes=[self.xres_b[s][j]])

        def p2(j):
            b = j % 2
            self.rms_stats(xo.t[:, b, :], xo.b[b], xn.t[:, b, :], xn.b[b], st.t[:, j, 0:1], st.t[:, j, 1:2],
                           st.t[:, j, 2:3], st.b[j])
            self.stt(xn.t[:, b, :], xo.t[:, b, :], st.t[:, j, 2:3], grow2.t[:], ALU.mult, ALU.mult,
                     r=[xo.b[b], st.b[j], grow2.B], w=[xn.b[b]])
            self.cp(h2b.t[:, b, :], xn.t[:, b, :], r=[xn.b[b]], w=[h2b.b[b]], eng="act")
            S.dma("sp", self.h2d[s][j * 128:(j + 1) * 128, :], h2b.t[:, b, :], h2b.b[b], reads=[h2b.b[b]],
                  writes=[self.h2d_b[s][j]])

        def p3(j):
            b = j % 2
            for kc in range(8):
                bk = 4 if kc < 4 else 5
                self.tr(ps[bk][:, (kc % 4) * 128:(kc % 4 + 1) * 128], xn.t[:, b, kc * 128:(kc + 1) * 128],
                        self.CF(CF_ID, 128), r=[xn.b[b], self.cf.B], w=[pb[bk]])
            self.cp(h2T.t[:, b, 0:4, :], ps[4][:, :].rearrange("p (k t) -> p k t", k=4), r=[pb[4]], w=[h2T.b[b]],
                    eng="act")
            self.cp(h2T.t[:, b, 4:8, :], ps[5][:, :].rearrange("p (k t) -> p k t", k=4), r=[pb[5]], w=[h2T.b[b]],
                    eng="dve")
            lbk = 6 + j % 2
            for kc in range(8):
                self.mm(ps[lbk][:, 0:16], h2T.t[:, b, kc, :], self.CF(lb + LP_WR + kc * 16, 16), kc == 0, kc == 7,
                        r=[h2T.b[b], self.cf.B], w=[pb[lbk]])

        def p4(j):
            b = j % 2
            lbk = 6 + j % 2
            nc_ = self.nc
            self.S.op("dve", lambda: nc_.vector.reduce_max(out=sm.t[:, j, 0:1], in_=ps[lbk][:, 0:16], axis=AX.X),
                      [pb[lbk]], [sm.b[j]])
            self.tsc(sm.t[:, j, 1:2], sm.t[:, j, 0:1], -1.0, None, ALU.mult, r=[sm.b[j]], w=[sm.b[j]])
            self.act(lg.t[:, b, :], ps[lbk][:, 0:16], AF.Exp, r=[pb[lbk], sm.b[j]], w=[lg.b[b], sm.b[j]],
                     bias=sm.t[:, j, 1:2], accum_out=sm.t[:, j, 2:3])
            self.recip(sm.t[:, j, 3:4], sm.t[:, j, 2:3], r=[sm.b[j]], w=[sm.b[j]])
            self.tsc(aff2.t[:, j, s * 16:(s + 1) * 16], lg.t[:, b, :], sm.t[:, j, 3:4], None, ALU.mult,
                     r=[lg.b[b], sm.b[j]], w=[aff2.B])

        for i in range(NT + 3):
            if i < NT:
                p1(i)
            if 0 <= i - 1 < NT:
                p2(i - 1)
            if 0 <= i - 2 < NT:
                p3(i - 2)
            if 0 <= i - 3 < NT:
                p4(i - 3)
        sc.close()
        if "xmid" in self.dbg:
            self.dump("xmid", self.xres[s], [S_LEN, D], F32)
        self.check_stop("M4")
        M.close()

    def phase_E(self, l):
        nc, S = self.nc, self.S
        ps, pb, psb, pbb = self.ps, self.pb, self.psb, self.pbb
        lb = CF_L0 + l * LP
        last = l == DEPTH - 1
        nseq = self.nseq
        E = Scope(self)
        epsc = E.sb("epsc", [128, 1], F32)
        self.memset(epsc.t[:], EPS, w=[epsc.B])
        self.epsc = epsc.t[:, 0:1]
        self.epsc_b = epsc.B
        aff2 = self.aff2
        mask2 = E.sb("mask2", [128, NT, 32], F32)
        gsel2 = E.sb("gsel2", [128, NT, 32], F32)
        rank2 = E.sb("rank2", [128, NT, 32], F32)
        self.dump("aff2", aff2.t[:], [128, NT, 32], F32)
        self.check_stop("Eroute")

        sc = Scope(self)
        affT = sc.sb("affT", [128, 512], F32)
        mask128 = sc.sb("mask128", [128, 512], F32)
        junk = sc.sb("junk", [128, 512], BF16)
        thr = sc.sb("thr", [128, 4], F32)
        maskb = sc.sb("maskb", [128, NT, 32], BF16)
        Tsb = sc.sb("Tsb", [128, NT, 32], F32)
        cum = sc.sb("cum", [128, NT, 32], F32)
        for g4 in range(4):
            self.tr(ps[0][:, g4 * 128:(g4 + 1) * 128], aff2.t[:, g4 * 4:(g4 + 1) * 4, :].rearrange("p j c -> p (j c)"),
                    self.CF(CF_ID, 128), r=[aff2.B, self.cf.B], w=[pb[0]])
        self.cp(affT.t[:], ps[0][:, :], r=[pb[0]], w=[affT.B], eng="dve")
        self.memset(thr.t[:], 0.0, w=[thr.B])
        self.check_stop("Es1")
        for it in range(26):
            step = 2.0 ** (-(it + 1))
            self.tsc(thr.t[:, 1:2], thr.t[:, 0:1], step, None, ALU.add, r=[thr.B], w=[thr.B])
            self.tsc(junk.t[:], affT.t[:], thr.t[:, 1:2], None, ALU.is_ge, ALU.add, r=[affT.B, thr.B],
                     w=[junk.B, thr.B], accum_out=thr.t[:, 2:3])
            self.mm(ps[1][:, 0:2], self.CF(CF_BM, 128), thr.t[:, 2:4], True, True, r=[thr.B, self.cf.B], w=[pb[1]])
            self.tsc(thr.t[:, 3:4], ps[1][:, 0:1], CAP - 0.5, thr.t[:, 1:2], ALU.is_ge, ALU.mult, r=[thr.B, pb[1]],
                     w=[thr.B])
            self.tt(thr.t[:, 0:1], thr.t[:, 0:1], thr.t[:, 3:4], ALU.max, r=[thr.B], w=[thr.B])
            if it == 0:
                self.check_stop("Es2")
        self.check_stop("Es3")
        self.tsc(mask128.t[:], affT.t[:], thr.t[:, 0:1], None, ALU.is_ge, r=[affT.B, thr.B], w=[mask128.B])
        for g4 in range(4):
            self.tr(ps[4][:, g4 * 128:(g4 + 1) * 128], mask128.t[:, g4 * 128:(g4 + 1) * 128], self.CF(CF_ID, 128),
                    r=[mask128.B, self.cf.B], w=[pb[4]])
        self.check_stop("Es4")
        m2f = mask2.t[:].rearrange("p j c -> p (j c)")
        self.cp(m2f, ps[4][:, :], r=[pb[4]], w=[mask2.B], eng="dve")
        self.cp(maskb.t[:].rearrange("p j c -> p (j c)"), m2f, r=[mask2.B], w=[maskb.B], eng="act")
        self.tt(gsel2.t[:], aff2.t[:], mask2.t[:], ALU.mult, r=[aff2.B, mask2.B], w=[gsel2.B])
        self.check_stop("Es5")
        mbf = maskb.t[:].rearrange("p j c -> p (j c)")
        self.mm(ps[5][:, :], self.CB(CB_TS, 128), mbf, True, True, r=[maskb.B, self.cb.B], w=[pb[5]])
        self.mm(ps[6][:, :], self.CB(CB_ONE, 128), mbf, True, True, r=[maskb.B, self.cb.B], w=[pb[6]])
        self.check_stop("Es6")
        self.cp(Tsb.t[:].rearrange("p j c -> p (j c)"), ps[6][:, :], r=[pb[6]], w=[Tsb.B], eng="act")
        self.memset(cum.t[:, 0, :], 0.0, w=[cum.B])
        for j in range(1, NT):
            self.tt(cum.t[:, j, :], cum.t[:, j - 1, :], Tsb.t[:, j - 1, :], ALU.add, r=[cum.B, Tsb.B], w=[cum.B])
        self.tt(rank2.t[:].rearrange("p j c -> p (j c)"), ps[5][:, :], cum.t[:].rearrange("p j c -> p (j c)"), ALU.add,
                r=[pb[5], cum.B], w=[rank2.B])
        self.check_stop("Es7")
        sc.close()
        self.dump("mask2", mask2.t[:], [128, NT, 32], F32)
        self.dump("rank2", rank2.t[:], [128, NT, 32], F32)
        self.check_stop("Esel")

        I32 = mybir.dt.int32
        idx_all = E.sb("idx_all", [128, NSEQ, 2 * NEXP], I32)
        g_all = E.sb("g_all", [128, NSEQ, 2 * NEXP], F32)
        sc = Scope(self)
        R = sc.sb("R", [128, NT, 32, 4], BF16)
        tokc = self.CB(CB_TOK, 2 * NT).rearrange("p (j o k) -> p j o k", j=NT, o=1)
        self.cp(R.t[:, :, :, 0:2], tokc.to_broadcast([128, NT, 32, 2]), r=[self.cb.B], w=[R.B], eng="dve")
        self.cp(R.t[:, :, :, 2:3], aff2.t[:].rearrange("p j (c o) -> p j c o", o=1), r=[aff2.B], w=[R.B], eng="dve")
        self.tt(R.t[:, :, :, 3:4], aff2.t[:].rearrange("p j (c o) -> p j c o", o=1), R.t[:, :, :, 2:3], ALU.subtract,
                r=[aff2.B, R.B], w=[R.B])
        Sel = sc.sb("Sel", [128, 2, NT, CAP], BF16, nb=2)
        ig = sc.sb("ig", [128, 2 * NEXP, 4], F32)
        igf = sc.sb("igf", [128, 2 * NEXP], F32)
        for s in range(nseq):
            for e in range(NEXP):
                col = s * 16 + e
                b = e % 2
                for j in range(NT):
                    self.tsc(Sel.t[:, b, j, :], self.CF(CF_IOTA, CAP), rank2.t[:, j, col:col + 1],
                             mask2.t[:, j, col:col + 1], ALU.is_equal, ALU.mult, r=[self.cf.B, rank2.B, mask2.B],
                             w=[Sel.b[b]])
                for ch in range(2):
                    i = e * 2 + ch
                    for j in range(NT):
                        self.mm(ps[s][:, i * 4:(i + 1) * 4], Sel.t[:, b, j, ch * 128:(ch + 1) * 128], R.t[:, j, col, :],
                                j == 0, j == NT - 1, r=[Sel.b[b], R.B], w=[pb[s]])
            self.cp(ig.t[:].rearrange("p i k -> p (i k)"), ps[s][:, 0:128], r=[pb[s]], w=[ig.B], eng="dve")
            self.tt(igf.t[:], ig.t[:, :, 0], ig.t[:, :, 1], ALU.add, r=[ig.B], w=[igf.B])
            self.cp(idx_all.t[:, s, :], igf.t[:], r=[igf.B], w=[idx_all.B], eng="dve")
            self.tt(g_all.t[:, s, :], ig.t[:, :, 2], ig.t[:, :, 3], ALU.add, r=[ig.B], w=[g_all.B])
        sc.close()
        self.dump("idx_all", idx_all.t[:], [128, NSEQ, 2 * NEXP], I32)
        self.dump("g_all", g_all.t[:], [128, NSEQ, 2 * NEXP], F32)
        self.check_stop("Eidx")

        sc = Scope(self)
        xtok = sc.sb("xtok", [128, 2, 4, D], BF16, nb=2)
        xeb = sc.sb("xeb", [128, 2, 8, 512], BF16, nb=2)
        wg = sc.sb("wg", [128, 2, 8, 512], BF16, nb=2)
        wu = sc.sb("wu", [128, 2, 8, 512], BF16, nb=2)
        wdn = sc.sb("wdn", [128, 2, 16, 512], BF16, nb=2)
        hid = sc.sb("hid", [128, 16, 512], BF16, nb=16)
        sgl = sc.sb("sgl", [128, 2, 512], F32, nb=2)
        yst = sc.sb("yst", [128, 2, 4, D], F32, nb=2)
        self.memset(xeb.t[:], 0.0, w=[xeb.b[0], xeb.b[1]])
        IO = bass.IndirectOffsetOnAxis

        def emit_gather(e):
            be = e % 2
            for cc in range(4):
                s2, ch = cc // 2, cc % 2
                if s2 < nseq:
                    S.idma(xtok.t[:, be, cc, :], self.h2d[s2], xtok.b[be],
                           in_off=IO(ap=idx_all.t[:, s2, e * 2 + ch:e * 2 + ch + 1], axis=0),
                           reads=self.h2d_b[s2] + [idx_all.B], writes=[xtok.b[be]])

        def emit_scatter(e):
            be = e % 2
            for cc in range(4):
                s2, ch = cc // 2, cc % 2
                if s2 < nseq:
                    S.idma(self.xres[s2], yst.t[:, be, cc, :], yst.b[be],
                           out_off=IO(ap=idx_all.t[:, s2, e * 2 + ch:e * 2 + ch + 1], axis=0),
                           reads=[yst.b[be], idx_all.B], writes=self.xres_b[s2], compute_op=ALU.add)

        def emit_xpose(e):
            be = e % 2
            for cc in range(4):
                if cc // 2 >= nseq:
                    continue
                g = cc % 2
                for kc in range(8):
                    self.tr(psb[g][:, kc * 128:(kc + 1) * 128], xtok.t[:, be, cc, kc * 128:(kc + 1) * 128],
                            self.CB(CB_ID, 128), r=[xtok.b[be], self.cb.B], w=[pbb[g]])
                self.cp(xeb.t[:, be, :, cc * 128:(cc + 1) * 128], psb[g][:, :].rearrange("p (k c) -> p k c", k=8),
                        r=[pbb[g]], w=[xeb.b[be]], eng="act" if cc % 2 else "dve")

        emit_gather(0)
        for e in range(NEXP):
            be = e % 2
            wge = self.w_gate_e[l, e].rearrange("(kc p) f -> p kc f", p=128)
            wue = self.w_up_e[l, e].rearrange("(kc p) f -> p kc f", p=128)
            wde = self.w_down_e[l, e].rearrange("(fc p) d -> p fc d", p=128)
            emit_xpose(e)
            if e + 1 < NEXP:
                emit_gather(e + 1)
            for fg in range(4):
                wb = (e * 4 + fg) % 2
                fs = slice(fg * 512, (fg + 1) * 512)
                S.dma("pool", wg.t[:, wb, :, :], wge[:, :, fs], wg.b[wb], writes=[wg.b[wb]])
                S.dma("pool", wu.t[:, wb, :, :], wue[:, :, fs], wu.b[wb], writes=[wu.b[wb]])
                if fg == 1 and e >= 1:
                    emit_scatter(e - 1)
                for fc in range(4):
                    f = fg * 4 + fc
                    pk = (f % 2) * 2
                    for kc in range(8):
                        self.mm(ps[pk][:, :], wg.t[:, wb, kc, fc * 128:(fc + 1) * 128], xeb.t[:, be, kc, :], kc == 0,
                                kc == 7, r=[wg.b[wb], xeb.b[be]], w=[pb[pk]])
                    for kc in range(8):
                        self.mm(ps[pk + 1][:, :], wu.t[:, wb, kc, fc * 128:(fc + 1) * 128], xeb.t[:, be, kc, :], kc == 0,
                                kc == 7, r=[wu.b[wb], xeb.b[be]], w=[pb[pk + 1]])
                    self.act(sgl.t[:, f % 2, :], ps[pk][:, :], AF.Silu, r=[pb[pk]], w=[sgl.b[f % 2]])
                    self.tt(hid.t[:, f, :], ps[pk + 1][:, :], sgl.t[:, f % 2, :], ALU.mult, r=[pb[pk + 1], sgl.b[f % 2]],
                            w=[hid.b[f]])
            for dh in range(2):
                wb2 = (e * 2 + dh) % 2
                S.dma("pool", wdn.t[:, wb2, :, :], wde[:, :, dh * 512:(dh + 1) * 512], wdn.b[wb2], writes=[wdn.b[wb2]])
                for cc in range(4):
                    bank = 4 + cc % 2
                    s2, ch = cc // 2, cc % 2
                    for fc in range(16):
                        self.mm(ps[bank][:, :], hid.t[:, fc, cc * 128:(cc + 1) * 128], wdn.t[:, wb2, fc, :], fc == 0,
                                fc == 15, r=[hid.b[fc], wdn.b[wb2]], w=[pb[bank]])
                    gcol = g_all.t[:, min(s2, nseq - 1), e * 2 + ch:e * 2 + ch + 1]
                    if cc % 2:
                        self.act(yst.t[:, be, cc, dh * 512:(dh + 1) * 512], ps[bank][:, :], AF.Copy,
                                 r=[pb[bank], g_all.B], w=[yst.b[be]], scale=gcol)
                    else:
                        self.tsc(yst.t[:, be, cc, dh * 512:(dh + 1) * 512], ps[bank][:, :], gcol, None, ALU.mult,
                                 r=[pb[bank], g_all.B], w=[yst.b[be]])
        emit_scatter(NEXP - 1)
        sc.close()
        self.check_stop("Effn")

        if last:
            sc = Scope(self)
            grow = sc.sb("grow", [128, D], F32)
            S.dma("sp", grow.t[:], self.grows[4], grow.B, writes=[grow.B])
            xin = sc.sb("xin", [128, 2, D], F32, nb=2)
            xf = sc.sb("xf", [128, 2, D], F32, nb=2)
            st = sc.sb("st", [128, NSEQ * NT, 4], F32, nb=NSEQ * NT)
            for s in range(nseq):
                for j in range(NT):
                    b = j % 2
                    k_ = s * NT + j
                    S.dma("sp", xin.t[:, b, :], self.xres[s][j * 128:(j + 1) * 128, :], xin.b[b],
                          reads=[self.xres_b[s][j]], writes=[xin.b[b]])
                    self.rms_stats(xin.t[:, b, :], xin.b[b], xf.t[:, b, :], xf.b[b], st.t[:, k_, 0:1], st.t[:, k_, 1:2],
                                   st.t[:, k_, 2:3], st.b[k_])
                    self.stt(xf.t[:, b, :], xin.t[:, b, :], st.t[:, k_, 2:3], grow.t[:], ALU.mult, ALU.mult,
                             r=[xin.b[b], st.b[k_], grow.B], w=[xf.b[b]])
                    S.dma("sp", self.y[s, j * 128:(j + 1) * 128, :], xf.t[:, b, :], xf.b[b], reads=[xf.b[b]])
            sc.close()
        E.close()
        if "xe%d" % l in self.dbg:
            self.dump("xe%d" % l, self.xres[0] if not last else self.y[0], [S_LEN, D], F32)
        self.check_stop("E%d" % l)


def host_consts(inp):
    cf = np.zeros((128, NCF), np.float32)
    p = np.arange(128)
    cf[:, CF_ID:CF_ID + 128] = np.eye(128)
    cf[:, CF_TL:CF_TL + 128] = (p[:, None] <= p[None, :])
    cf[:, CF_TU:CF_TU + 128] = (p[:, None] >= p[None, :])
    cf[:, CF_ONE:CF_ONE + 128] = 1.0
    cf[:, CF_IOTA:CF_IOTA + 256] = np.arange(256)[None, :]
    for l in range(DEPTH):
        lb = CF_L0 + l * LP
        cf[:, lb + LP_BMG:lb + LP_BMG + 256] = np.tile(inp["b_mgate"][l], NT)[None, :]
        cw = inp["conv_w"][l]
        cf[:, lb + LP_CW:lb + LP_CW + 40] = cw.reshape(5, 8, 128).transpose(2, 1, 0).reshape(128, 40)
        cf[:, lb + LP_CB:lb + LP_CB + 8] = inp["conv_b"][l].reshape(8, 128).T
        cf[:, lb + LP_MNG:lb + LP_MNG + 512] = inp["mlstm_norm_g"][l][None, :]
        cf[:, lb + LP_DNG:lb + LP_DNG + 128] = inp["diff_norm_g"][l][None, :]
        cf[:, lb + LP_LAM:lb + LP_LAM + 256] = inp["diff_lam"][l].reshape(256)[None, :]
        cf[:, lb + LP_WR:lb + LP_WR + 128] = inp["w_router"][l].reshape(8, 128, 16).transpose(1, 0, 2).reshape(128, 128)
    cf[:, CF_BM:CF_BM + 128] = ((p[:, None] % 32) == (p[None, :] % 32))
    cb = np.zeros((128, NCB), np.float32)
    cb[:, CB_ID:CB_ID + 128] = np.eye(128)
    cb[:, CB_TL:CB_TL + 128] = (p[:, None] <= p[None, :])
    cb[:, CB_TU:CB_TU + 128] = (p[:, None] >= p[None, :])
    cb[:, CB_TS:CB_TS + 128] = (p[:, None] < p[None, :])
    cb[:, CB_ONE:CB_ONE + 128] = 1.0
    for j in range(NT):
        cb[:, CB_TOK + 2 * j] = 128.0 * j
        cb[:, CB_TOK + 2 * j + 1] = p
    pos = np.arange(S_LEN)
    hi, lo = (pos // 128).astype(np.float32), (pos % 128).astype(np.float32)
    kaug = np.zeros((2, 32, S_LEN), np.float32)
    kaug[0, 0], kaug[0, 1], kaug[0, 2], kaug[0, 3] = -1.0, -1.0, 128.0 * hi, lo
    kaug[1] = -kaug[0]
    qaug = np.zeros((4, 32, S_LEN), np.float32)
    for h in range(4):
        sl = 2.0 ** (-2.0 * (h + 1))
        qaug[h, 0], qaug[h, 1], qaug[h, 2], qaug[h, 3] = sl * 128.0 * hi, sl * lo, sl, sl
        kq = p[None, :].astype(np.float32) - p[:, None].astype(np.float32)
        cb[:, CB_CORR + h * 128:CB_CORR + (h + 1) * 128] = np.where(kq < 0, 2.0 * sl * kq, 0.0)
    grows = np.stack([np.broadcast_to(v[None, :], (128, D)) for v in
                      (inp["norm_mix_g"][0], inp["norm_mix_g"][1], inp["norm_ffn_g"][0], inp["norm_ffn_g"][1],
                       inp["norm_f_g"])]).astype(np.float32)
    bf = ml_dtypes.bfloat16
    return dict(cf=cf, cb=cb.astype(bf), grows=np.ascontiguousarray(grows), kaug=kaug.astype(bf), qaug=qaug.astype(bf))


def make_in_maps(inp, ncores=NCORES, use_moe=True):
    inp = {k: np.asarray(v) for k, v in inp.items()}
    hc = host_consts(inp)
    shared = dict(w_in=inp["w_in"], w_up_a=inp["w_up_a"], w_up_b=inp["w_up_b"], w_out=inp["w_out"], **hc)
    if use_moe:
        shared.update(w_gate_e=inp["w_gate_e"], w_up_e=inp["w_up_e"], w_down_e=inp["w_down_e"])
    maps = []
    for c in range(ncores):
        m = dict(shared)
        m["x"] = np.ascontiguousarray(inp["x"][c * NSEQ:(c + 1) * NSEQ])
        maps.append(m)
    return maps


def kernel(**inputs):
    k = K()
    nc = k.build()
    maps = make_in_maps(inputs)
    res = run_bass_kernel_spmd(nc, maps, core_ids=list(range(NCORES)))
    return np.concatenate([np.asarray(r["y"]) for r in res.results], axis=0).astype(np.float32)
```

```python
import math
from contextlib import ExitStack

import numpy as np
import ml_dtypes
import concourse.bass as bass
import concourse.mybir as mybir
from concourse.alu_op_type import AluOpType as ALU
from concourse.bass_utils import run_bass_kernel_spmd

F32, BF16 = mybir.dt.float32, mybir.dt.bfloat16
AF = mybir.ActivationFunctionType
AX = mybir.AxisListType

NCORES = 8
NSEQ = 2
S_LEN = 2048
D = 1024
NT = 16
DEPTH = 2
IN_COLS = 5648
OFF_MG = 2048
OFF_DQ = 2064
OFF_DK = 2576
OFF_DV = 3088
OFF_GATE = 3600
NEXP = 16
CAP = 256
EPS = 1e-6

CF_ID, CF_TL, CF_TU, CF_ONE, CF_IOTA = 0, 128, 256, 384, 512
CF_L0 = 768
LP_BMG, LP_CW, LP_CB, LP_MNG, LP_DNG, LP_LAM, LP_WR = 0, 256, 296, 304, 816, 944, 1200
LP = 1328
CF_BM = CF_L0 + DEPTH * LP
NCF = CF_BM + 128
CB_ID, CB_TL, CB_TU, CB_TS, CB_ONE, CB_CORR = 0, 128, 256, 384, 512, 640
CB_TOK = 640 + 4 * 128
CB_IOTA = CB_TOK + 2 * NT
NCB = CB_IOTA + CAP


class Buf:
    __slots__ = ("name", "w", "r", "dsem", "dcnt")

    def __init__(self, name):
        self.name = name
        self.w = None
        self.r = []
        self.dsem = None
        self.dcnt = 0


class Tok:
    __slots__ = ("sem", "val", "holder", "eng")

    def __init__(self, sem, val, holder=None, eng=None):
        self.sem, self.val, self.holder, self.eng = sem, val, holder, eng


class Sched:
    def __init__(self, nc):
        self.nc = nc
        self.E = {"pe": nc.tensor, "act": nc.scalar, "dve": nc.vector, "pool": nc.gpsimd, "sp": nc.sync}
        self.sem = {e: nc.alloc_semaphore("sem_" + e) for e in self.E}
        self.cnt = {e: 0 for e in self.E}
        self.known = {e: {} for e in self.E}
        self.dbufs = []
        self.free_dsems_k = {}
        self.dkind = {}
        self.nwait = 0
        self.nins = 0

    def _wait(self, e, toks):
        need = {}
        for t in toks:
            if t is None:
                continue
            v = t.holder.dcnt if t.holder is not None else t.val
            k = id(t.sem)
            if k not in need or need[k][1] < v:
                need[k] = (t.sem, v, t)
        for k, (sem, v, t) in need.items():
            if e == "pe" and t.eng == "pe":
                continue
            if self.known[e].get(k, 0) >= v:
                continue
            self.E[e].wait_ge(sem, v)
            self.known[e][k] = v
            self.nwait += 1

    def _deps(self, reads, writes):
        toks = []
        for b in reads:
            toks.append(b.w)
        for b in writes:
            toks.append(b.w)
            toks.extend(b.r)
        return toks

    def _mark(self, tok, reads, writes):
        for b in reads:
            b.r = [t for t in b.r if t.sem is not tok.sem] + [tok]
        for b in writes:
            b.w = tok
            b.r = []

    def op(self, e, fn, reads=(), writes=()):
        self._wait(e, self._deps(reads, writes))
        ins = fn()
        self.cnt[e] += 1
        ins.then_inc(self.sem[e], 1)
        self.nins += 1
        self._mark(Tok(self.sem[e], self.cnt[e], None, e), reads, writes)

    def dma(self, q, out, in_, holder, reads=(), writes=(), **kw):
        self._wait(q, self._deps(reads, writes))
        self._dsem(q, holder)
        ins = self.E[q].dma_start(out=out, in_=in_, **kw)
        holder.dcnt += 16
        ins.then_inc(holder.dsem, 16)
        self.nins += 1
        self._mark(Tok(holder.dsem, holder.dcnt, holder, "dma"), reads, writes)

    def _dsem(self, q, holder):
        kind = "sw" if q == "pool" else "hw"
        if holder.dsem is None:
            fl = self.free_dsems_k.setdefault(kind, [])
            if fl:
                holder.dsem, holder.dcnt = fl.pop()
            else:
                self.nsem = getattr(self, "nsem", 0) + 1
                holder.dsem = self.nc.alloc_semaphore("dsem_%d" % self.nsem)
            self.dbufs.append(holder)
            self.dkind[id(holder)] = kind
        assert self.dkind[id(holder)] == kind, "buffer mixes SW and HW DMA queues"

    def idma(self, out, in_, holder, out_off=None, in_off=None, reads=(), writes=(), **kw):
        self._wait("pool", self._deps(reads, writes))
        self._dsem("pool", holder)
        ins = self.nc.gpsimd.indirect_dma_start(out=out, out_offset=out_off, in_=in_, in_offset=in_off, **kw)
        holder.dcnt += 16
        ins.then_inc(holder.dsem, 16)
        self.nins += 1
        self._mark(Tok(holder.dsem, holder.dcnt, holder, "dma"), reads, writes)

    def recycle(self, bufs):
        for b in bufs:
            if b.dsem is not None and b in self.dbufs:
                self.dbufs.remove(b)
                self.free_dsems_k[self.dkind[id(b)]].append((b.dsem, b.dcnt))

    def barrier(self):
        for e in self.E:
            toks = [Tok(self.sem[e2], self.cnt[e2], None, "x") for e2 in self.E if self.cnt[e2] > 0]
            toks += [Tok(b.dsem, b.dcnt, b, "dma") for b in self.dbufs]
            self._wait(e, toks)


class Tile:
    def __init__(self, t, nb, name):
        self.t = t
        self.b = [Buf("%s.%d" % (name, i)) for i in range(nb)]
        self.B = self.b[0]


class Scope:
    def __init__(self, K):
        self.K = K
        self.st = ExitStack()
        self.tiles = []

    def sb(self, name, shape, dt, nb=1):
        self.K.uid += 1
        nm = "%s_%d" % (name, self.K.uid)
        t = self.st.enter_context(self.K.nc.sbuf_tensor(nm, list(shape), dt))
        tl = Tile(t, nb, nm)
        self.tiles.append(tl)
        return tl

    def close(self):
        self.K.S.barrier()
        for tl in self.tiles:
            self.K.S.recycle(tl.b)
        self.st.close()


class K:
    def __init__(self, dbg=None, stop=None, nseq=NSEQ):
        self.dbg = dbg or []
        self.stop = stop
        self.nseq = nseq
        self.uid = 0
        self.dumps = {}
        nc = self.nc = bass.Bass("TRN2", target_bir_lowering=False)
        self.S = Sched(nc)
        dt = nc.dram_tensor
        self.x = dt("x", [NSEQ, S_LEN, D], F32, kind="ExternalInput").ap()
        self.w_in = dt("w_in", [DEPTH, D, IN_COLS], F32, kind="ExternalInput").ap()
        self.w_up_a = dt("w_up_a", [DEPTH, 512, D], F32, kind="ExternalInput").ap()
        self.w_up_b = dt("w_up_b", [DEPTH, 512, D], F32, kind="ExternalInput").ap()
        self.w_out = dt("w_out", [DEPTH, D, D], F32, kind="ExternalInput").ap()
        self.use_moe = stop not in ("M0", "M1", "M3", "M4", "Eroute", "Es1", "Es2", "Es3", "Es4", "Es5", "Es6", "Es7", "Esel", "Eidx", "Egather")
        if self.use_moe:
            self.w_gate_e = dt("w_gate_e", [DEPTH, NEXP, D, 2 * D], F32, kind="ExternalInput").ap()
            self.w_up_e = dt("w_up_e", [DEPTH, NEXP, D, 2 * D], F32, kind="ExternalInput").ap()
            self.w_down_e = dt("w_down_e", [DEPTH, NEXP, 2 * D, D], F32, kind="ExternalInput").ap()
        self.cf_d = dt("cf", [128, NCF], F32, kind="ExternalInput").ap()
        self.cb_d = dt("cb", [128, NCB], BF16, kind="ExternalInput").ap()
        self.grows = dt("grows", [5, 128, D], F32, kind="ExternalInput").ap()
        self.kaug = dt("kaug", [2, 32, S_LEN], BF16, kind="ExternalInput").ap()
        self.qaug = dt("qaug", [4, 32, S_LEN], BF16, kind="ExternalInput").ap()
        self.y = dt("y", [NSEQ, S_LEN, D], F32, kind="ExternalOutput").ap()
        self.xres = [dt("xres%d" % i, [S_LEN, D], F32).ap() for i in range(NSEQ)]
        self.h2d = [dt("h2d%d" % i, [S_LEN, D], BF16).ap() for i in range(NSEQ)]
        self.xed = dt("xed", [NEXP, NSEQ, 128, 8, CAP], BF16).ap()
        self.yed = dt("yed", [NSEQ, 2 * NEXP, 128, D], BF16).ap()
        self.xres_b = [[Buf("xres%d_%d" % (s, j)) for j in range(NT)] for s in range(NSEQ)]

    def mm(self, out, lhsT, rhs, start, stop, r=(), w=()):
        nc = self.nc
        self.S.op("pe", lambda: nc.tensor.matmul(out, lhsT=lhsT, rhs=rhs, start=start, stop=stop), r, w)

    def tr(self, out, in_, ident, r=(), w=()):
        nc = self.nc
        self.S.op("pe", lambda: nc.tensor.transpose(out, in_, ident), r, w)

    def act(self, out, in_, func, r=(), w=(), **kw):
        nc = self.nc
        self.S.op("act", lambda: nc.scalar.activation(out=out, in_=in_, func=func, **kw), r, w)

    def tsc(self, out, in0, s1, s2, op0, op1=None, r=(), w=(), eng="dve", accum_out=None):
        E = self.nc.vector if eng == "dve" else self.nc.gpsimd
        kw = {}
        if op1 is not None:
            kw["op1"] = op1
        if accum_out is not None:
            kw["accum_out"] = accum_out
        self.S.op(eng, lambda: E.tensor_scalar(out=out, in0=in0, scalar1=s1, scalar2=s2, op0=op0, **kw), r, w)

    def tt(self, out, in0, in1, op, r=(), w=(), eng="dve"):
        E = self.nc.vector if eng == "dve" else self.nc.gpsimd
        self.S.op(eng, lambda: E.tensor_tensor(out=out, in0=in0, in1=in1, op=op), r, w)

    def stt(self, out, in0, scalar, in1, op0, op1, r=(), w=()):
        nc = self.nc
        self.S.op("dve", lambda: nc.vector.scalar_tensor_tensor(out=out, in0=in0, scalar=scalar, in1=in1,
                                                                 op0=op0, op1=op1), r, w)

    def cp(self, out, in_, r=(), w=(), eng="dve"):
        if eng == "act":
            nc = self.nc
            self.S.op("act", lambda: nc.scalar.copy(out=out, in_=in_), r, w)
        else:
            E = self.nc.vector if eng == "dve" else self.nc.gpsimd
            self.S.op(eng, lambda: E.tensor_copy(out=out, in_=in_), r, w)

    def recip(self, out, in_, r=(), w=()):
        nc = self.nc
        self.S.op("dve", lambda: nc.vector.reciprocal(out=out, in_=in_), r, w)

    def memset(self, ap, val, w=(), eng="dve"):
        E = self.nc.vector if eng == "dve" else self.nc.gpsimd
        self.S.op(eng, lambda: E.memset(ap, val), (), w)

    def dump(self, name, src_ap, shape, dtype, rbufs=()):
        if name not in self.dbg:
            return
        self.S.barrier()
        d = self.nc.dram_tensor("dbg_" + name, list(shape), dtype, kind="ExternalOutput").ap()
        hb = Buf("dbg_" + name)
        self.S.dma("sp", d, src_ap, hb, reads=rbufs)
        self.S.barrier()
        self.S.recycle([hb])
        self.dumps[name] = "dbg_" + name

    def build(self):
        nc, S = self.nc, self.S
        g = Scope(self)
        self.g = g
        self.cf = g.sb("cf", [128, NCF], F32)
        self.cb = g.sb("cb", [128, NCB], BF16)
        S.dma("sp", self.cf.t[:], self.cf_d, self.cf.B, writes=[self.cf.B])
        S.dma("sp", self.cb.t[:], self.cb_d, self.cb.B, writes=[self.cb.B])
        self.ps = []
        self.pb = []
        for i in range(8):
            self.ps.append(nc.alloc_psum_tensor("ps%d" % i, [128, 512], F32))
            self.pb.append(Buf("ps%d" % i))
        self.psb = [self.ps[6].bitcast(BF16), self.ps[7].bitcast(BF16)]
        self.pbb = [self.pb[6], self.pb[7]]
        self.aff2 = g.sb("aff2", [128, NT, 32], F32)
        self.memset(self.aff2.t[:], 0.0, w=[self.aff2.B])
        self.h2d_b = [[Buf("h2d") for j in range(NT)] for s in range(NSEQ)]
        self.xed_b = [[Buf("xed") for s in range(NSEQ)] for e in range(NEXP)]
        self.yed_b = [[Buf("yed") for i in range(2 * NEXP)] for s in range(NSEQ)]
        S.barrier()
        try:
            for l in range(DEPTH):
                for s in range(self.nseq):
                    self.phase_M(l, s)
                self.phase_E(l)
        except StopIteration:
            pass
        S.barrier()
        return nc

    def CF(self, c0, n):
        return self.cf.t[:, c0:c0 + n]

    def CB(self, c0, n):
        return self.cb.t[:, c0:c0 + n]

    def check_stop(self, name):
        if self.stop == name:
            raise StopIteration

    def rms_stats(self, xin_ap, xin_b, junk_ap, junk_b, ss_ap, sq_ap, rstd_ap, sb_, n=D):
        self.act(junk_ap, xin_ap, AF.Square, r=[xin_b], w=[junk_b, sb_], accum_out=ss_ap)
        self.act(sq_ap, ss_ap, AF.Sqrt, r=[sb_, self.epsc_b], w=[sb_], bias=self.epsc, scale=1.0 / n)
        self.recip(rstd_ap, sq_ap, r=[sb_], w=[sb_])

    def phase_M(self, l, s):
        nc, S = self.nc, self.S
        ps, pb, psb, pbb = self.ps, self.pb, self.psb, self.pbb
        lam_init = 0.8 - 0.6 * math.exp(-0.3 * l)
        lb = CF_L0 + l * LP
        M = Scope(self)
        hT = M.sb("hT", [128, 8, S_LEN], BF16)
        hmT = M.sb("hmT", [128, 4, S_LEN], BF16)
        odT = M.sb("odT", [128, 4, S_LEN], BF16)
        epsc = M.sb("epsc", [128, 1], F32)
        self.memset(epsc.t[:], EPS, w=[epsc.B])
        self.epsc = epsc.t[:, 0:1]
        self.epsc_b = epsc.B

        def xsrc(j):
            if l == 0:
                return self.x[s, j * 128:(j + 1) * 128, :], None
            return self.xres[s][j * 128:(j + 1) * 128, :], self.xres_b[s][j]

        sc = Scope(self)
        grow = sc.sb("grow", [128, D], F32)
        S.dma("sp", grow.t[:], self.grows[l], grow.B, writes=[grow.B])
        xin = sc.sb("xin", [128, 4, D], F32, nb=4)
        xn = sc.sb("xn", [128, 4, D], F32, nb=4)
        st = sc.sb("st", [128, NT, 4], F32, nb=NT)
        def m0_a(j):
            b = j % 4
            src, sbuf = xsrc(j)
            S.dma("sp", xin.t[:, b, :], src, xin.b[b], reads=[sbuf] if sbuf else [], writes=[xin.b[b]])
            self.rms_stats(xin.t[:, b, :], xin.b[b], xn.t[:, b, :], xn.b[b], st.t[:, j, 0:1], st.t[:, j, 1:2],
                           st.t[:, j, 2:3], st.b[j])
            self.stt(xn.t[:, b, :], xin.t[:, b, :], st.t[:, j, 2:3], grow.t[:], ALU.mult, ALU.mult,
                     r=[xin.b[b], st.b[j], grow.B], w=[xn.b[b]])

        def m0_b(j):
            b = j % 4
            pa, pc = (j % 2) * 2, (j % 2) * 2 + 1
            for kc in range(8):
                bk = pa if kc < 4 else pc
                self.tr(ps[bk][:, (kc % 4) * 128:(kc % 4 + 1) * 128], xn.t[:, b, kc * 128:(kc + 1) * 128],
                        self.CF(CF_ID, 128), r=[xn.b[b], self.cf.B], w=[pb[bk]])
            self.cp(hT.t[:, 0:4, j * 128:(j + 1) * 128], ps[pa][:, :].rearrange("p (k t) -> p k t", k=4),
                    r=[pb[pa]], w=[hT.B], eng="act")
            self.cp(hT.t[:, 4:8, j * 128:(j + 1) * 128], ps[pc][:, :].rearrange("p (k t) -> p k t", k=4),
                    r=[pb[pc]], w=[hT.B], eng="dve")

        for j in range(NT + 2):
            if j < NT:
                m0_a(j)
            if j >= 2:
                m0_b(j - 2)
        sc.close()
        self.dump("hT", hT.t[:], [128, 8, S_LEN], BF16)
        self.check_stop("M0")

        sc = Scope(self)
        wgt = sc.sb("wgt", [128, 8, 16], BF16)
        win_l = self.w_in[l].rearrange("(kc p) c -> p kc c", p=128)
        S.dma("pool", wgt.t[:], win_l[:, :, OFF_MG:OFF_MG + 16], wgt.B, writes=[wgt.B])
        for j in range(NT):
            for kc in range(8):
                self.mm(ps[0][:, j * 16:(j + 1) * 16], hT.t[:, kc, j * 128:(j + 1) * 128], wgt.t[:, kc, :],
                        kc == 0, kc == 7, r=[wgt.B], w=[pb[0]])
        GT = sc.sb("GT", [128, 256], F32)
        self.tt(GT.t[:], ps[0][:, 0:256], self.CF(lb + LP_BMG, 256), ALU.add, r=[pb[0], self.cf.B], w=[GT.B])
        GT5 = GT.t[:].rearrange("p (j a b h) -> p j a b h", j=NT, a=2, b=2, h=4)
        LI = sc.sb("LI", [128, 2, NT, 4], F32)
        LF = sc.sb("LF", [128, 2, NT, 4], F32)
        tmpg = sc.sb("tmpg", [128, 2, NT, 4], F32)
        v4 = lambda t: t.t[:].rearrange("p a j h -> p j a h")
        self.cp(v4(LI), GT5[:, :, :, 0, :], r=[GT.B], w=[LI.B])
        self.act(v4(tmpg), GT5[:, :, :, 1, :], AF.Exp, r=[GT.B], w=[tmpg.B], scale=-1.0)
        self.act(tmpg.t[:], tmpg.t[:], AF.Ln, r=[tmpg.B], w=[tmpg.B], bias=1.0)
        self.tsc(LF.t[:], tmpg.t[:], -1.0, None, ALU.mult, r=[tmpg.B], w=[LF.B])
        fl = lambda ap: ap.rearrange("p j h -> p (j h)")
        self.mm(ps[1][:, 0:64], self.CF(CF_TL, 128), fl(LF.t[:, 0, :, :]), True, True, r=[LF.B, self.cf.B], w=[pb[1]])
        self.mm(ps[1][:, 64:128], self.CF(CF_TU, 128), fl(LF.t[:, 1, :, :]), True, True, r=[LF.B, self.cf.B], w=[pb[1]])
        self.mm(ps[2][:, 0:128], self.CF(CF_ONE, 128), LF.t[:].rearrange("p a j h -> p (a j h)"), True, True,
                r=[LF.B, self.cf.B], w=[pb[2]])
        A = sc.sb("A", [128, 2, NT, 4], F32)
        EB = sc.sb("EB", [128, 2, NT, 4], F32)
        EG = sc.sb("EG", [128, 2, NT, 4], F32)
        f3 = lambda t: t.t[:].rearrange("p a j h -> p (a j h)")
        self.tt(f3(tmpg), f3(LI), ps[1][:, 0:128], ALU.subtract, r=[LI.B, pb[1]], w=[tmpg.B])
        lnk = sc.sb("lnk", [128, 1], F32)
        self.memset(lnk.t[:], math.log(128 ** -0.5), w=[lnk.B])
        self.act(A.t[:], tmpg.t[:], AF.Exp, r=[tmpg.B, lnk.B], w=[A.B], bias=lnk.t[:, 0:1])
        self.act(f3(EB), ps[1][:, 0:128], AF.Exp, r=[pb[1]], w=[EB.B])
        self.act(f3(EG), ps[2][:, 0:128], AF.Exp, r=[pb[2]], w=[EG.B])
        S.barrier()

        wh = sc.sb("wh", [128, 2, 8, 4, 128], BF16, nb=2)
        pre = sc.sb("pre", [128, 2, S_LEN + 4], BF16, nb=2)
        dg = sc.sb("dg", [128, 2, 5, 128], BF16, nb=2)
        qk = sc.sb("qk", [128, 2, S_LEN], BF16, nb=2)
        ktok = sc.sb("ktok", [128, NT, 128], BF16)
        V = [sc.sb("Vf", [128, NT, 129], BF16), sc.sb("Vb", [128, NT, 129], BF16)]
        sigo = sc.sb("sigo", [128, NT, 128], BF16)
        WT = [sc.sb("WTf", [128, NT, 128], BF16, nb=NT), sc.sb("WTb", [128, NT, 128], BF16, nb=NT)]
        Hs = [sc.sb("Hsf", [128, NT, 129], F32, nb=NT), sc.sb("Hsb", [128, NT, 129], F32, nb=NT)]
        Sf = [sc.sb("Sf", [128, 129], F32), sc.sb("Sb", [128, 129], F32)]
        tmpS = [sc.sb("tSf", [128, 129], F32), sc.sb("tSb", [128, 129], F32)]
        Sbf = [sc.sb("Sfb", [128, 129], BF16), sc.sb("Sbb", [128, 129], BF16)]
        den = sc.sb("den", [128, 2, NT, 2], F32)
        hs = sc.sb("hs", [128, NT, 128], F32, nb=NT)
        bst = sc.sb("bst", [128, NT, 6], F32, nb=NT)
        mv = sc.sb("mv", [128, NT, 4], F32)
        xh = sc.sb("xh", [128, 4, 128], F32, nb=4)
        hmt = sc.sb("hmt", [128, 4, 128], BF16, nb=4)
        nmr = sc.sb("nmr", [128, NT, 1], F32)
        self.memset(pre.t[:], 0.0, w=[pre.b[0], pre.b[1]])

        def load_wh(h_):
            for qi in range(4):
                c0 = qi * 512 + h_ * 128
                S.dma("pool", wh.t[:, h_ % 2, :, qi, :], win_l[:, :, c0:c0 + 128], wh.b[h_ % 2], writes=[wh.b[h_ % 2]])

        load_wh(0)
        for hd in range(4):
            for qi in range(2):
                cwb = lb + LP_CW + (qi * 4 + hd) * 5
                for tp in range(5):
                    self.tsc(dg.t[:, qi, tp, :], self.CB(CB_ID, 128), self.CF(cwb + tp, 1), None, ALU.mult,
                             r=[self.cb.B, self.cf.B], w=[dg.b[qi]])
                for n in range(4):
                    bk = qi * 4 + n
                    for kc in range(8):
                        self.mm(ps[bk][:, :], wh.t[:, hd % 2, kc, qi, :], hT.t[:, kc, n * 512:(n + 1) * 512], kc == 0, kc == 7,
                                r=[wh.b[hd % 2]], w=[pb[bk]])
                    self.cp(pre.t[:, qi, 2 + n * 512:2 + (n + 1) * 512], ps[bk][:, :], r=[pb[bk]], w=[pre.b[qi]], eng="act")
            for qi in range(2):
                for n in range(4):
                    bk = qi * 4 + n
                    for tp in range(5):
                        self.mm(ps[bk][:, :], dg.t[:, qi, tp, :], pre.t[:, qi, tp + n * 512:tp + (n + 1) * 512], tp == 0,
                                tp == 4, r=[dg.b[qi], pre.b[qi]], w=[pb[bk]])
                    self.act(qk.t[:, qi, n * 512:(n + 1) * 512], ps[bk][:, :], AF.Silu, r=[pb[bk], self.cf.B],
                             w=[qk.b[qi]], bias=self.CF(lb + LP_CB + qi * 4 + hd, 1))
            for j in range(NT):
                bk = j % 4
                for kc in range(8):
                    self.mm(ps[bk][:, 0:256], hT.t[:, kc, j * 128:(j + 1) * 128],
                            wh.t[:, hd % 2, kc, 2:4, :], kc == 0, kc == 7, r=[wh.b[hd % 2]], w=[pb[bk]])
                for dr in range(2):
                    self.act(V[dr].t[:, j, 0:128], ps[bk][:, 0:128], AF.Copy, r=[pb[bk], A.B], w=[V[dr].B],
                             scale=A.t[:, dr, j, hd:hd + 1])
                self.act(sigo.t[:, j, :], ps[bk][:, 128:256], AF.Sigmoid, r=[pb[bk]], w=[sigo.B])
            for dr in range(2):
                self.cp(V[dr].t[:, :, 128:129], A.t[:, dr, :, hd:hd + 1], r=[A.B], w=[V[dr].B])
            if hd + 1 < 4:
                load_wh(hd + 1)
            for j in range(NT):
                g8 = j // 8
                self.tr(psb[g8][:, (j % 8) * 128:(j % 8 + 1) * 128], qk.t[:, 1, j * 128:(j + 1) * 128],
                        self.CB(CB_ID, 128), r=[qk.b[1], self.cb.B], w=[pbb[g8]])
                if j % 8 == 7:
                    self.cp(ktok.t[:, g8 * 8:(g8 + 1) * 8, :].rearrange("p j d -> p (j d)"), psb[g8][:, :],
                            r=[pbb[g8]], w=[ktok.B], eng="act")
            for c in range(NT):
                bk = c % 2
                cs = slice(c * 128, (c + 1) * 128)
                self.mm(ps[bk][:, 0:128], qk.t[:, 1, cs], qk.t[:, 0, cs], True, True, r=[qk.b[0], qk.b[1]], w=[pb[bk]])
                self.tt(WT[0].t[:, c, :], ps[bk][:, 0:128], self.CB(CB_TL, 128), ALU.mult, r=[pb[bk], self.cb.B],
                        w=[WT[0].b[c]])
                self.tt(WT[1].t[:, c, :], ps[bk][:, 0:128], self.CB(CB_TU, 128), ALU.mult, r=[pb[bk], self.cb.B],
                        w=[WT[1].b[c]])
            for step in range(NT):
                for dr in range(2):
                    c = step if dr == 0 else NT - 1 - step
                    cp_ = c - 1 if dr == 0 else c + 1
                    cs = slice(c * 128, (c + 1) * 128)
                    hb = dr * 2 + step % 2
                    kb = 4 + dr * 2 + step % 2
                    gcol = dr * 4 + hd
                    if step > 0:
                        self.act(Sbf[dr].t[:], Sf[dr].t[:], AF.Copy, r=[Sf[dr].B, EG.B], w=[Sbf[dr].B],
                                 scale=EG.t[:, dr, cp_, hd:hd + 1])
                    self.mm(ps[hb][:, 0:129], WT[dr].t[:, c, :], V[dr].t[:, c, :], True, step == 0,
                            r=[WT[dr].b[c], V[dr].B], w=[pb[hb]])
                    if step > 0:
                        self.mm(ps[hb][:, 0:129], qk.t[:, 0, cs], Sbf[dr].t[:], False, True,
                                r=[qk.b[0], Sbf[dr].B], w=[pb[hb]])
                    self.tsc(Hs[dr].t[:, c, :], ps[hb][:, 0:129], EB.t[:, dr, c, hd:hd + 1], None, ALU.mult,
                             r=[pb[hb], EB.B], w=[Hs[dr].b[c]])
                    if step < NT - 1:
                        self.mm(ps[kb][:, 0:129], ktok.t[:, c, :], V[dr].t[:, c, :], True, True,
                                r=[ktok.B, V[dr].B], w=[pb[kb]])
                        if step == 0:
                            self.cp(Sf[dr].t[:], ps[kb][:, 0:129], r=[pb[kb]], w=[Sf[dr].B], eng="dve")
                        else:
                            self.stt(Sf[dr].t[:], Sf[dr].t[:], EG.t[:, dr, cp_, hd:hd + 1], ps[kb][:, 0:129],
                                     ALU.mult, ALU.add, r=[Sf[dr].B, EG.B, pb[kb]], w=[Sf[dr].B])
            for dr in range(2):
                self.stt(den.t[:, dr, :, 0:1], Hs[dr].t[:, :, 128:129], -1.0, Hs[dr].t[:, :, 128:129], ALU.mult, ALU.max,
                         r=Hs[dr].b, w=[den.B])
                self.tsc(den.t[:, dr, :, 0:1], den.t[:, dr, :, 0:1], 1.0, None, ALU.max, r=[den.B], w=[den.B])
                self.recip(den.t[:, dr, :, 1:2], den.t[:, dr, :, 0:1], r=[den.B], w=[den.B])
            nc_ = self.nc
            for j in range(NT):
                self.act(hs.t[:, j, :], Hs[0].t[:, j, 0:128], AF.Copy, r=[Hs[0].b[j], den.B], w=[hs.b[j]],
                         scale=den.t[:, 0, j, 1:2])
            for j in range(NT):
                self.stt(hs.t[:, j, :], Hs[1].t[:, j, 0:128], den.t[:, 1, j, 1:2], hs.t[:, j, :], ALU.mult, ALU.add,
                         r=[Hs[1].b[j], den.B, hs.b[j]], w=[hs.b[j]])
            for j in range(NT):
                self.S.op("dve", lambda: nc_.vector.bn_stats(out=bst.t[:, j, :], in_=hs.t[:, j, :]), [hs.b[j]], [bst.b[j]])
            for j in range(NT):
                self.S.op("dve", lambda: nc_.vector.bn_aggr(out=mv.t[:, j, 0:2], in_=bst.t[:, j, :]), [bst.b[j]], [mv.B])
            self.act(mv.t[:, :, 2:3], mv.t[:, :, 1:2], AF.Sqrt, r=[mv.B, self.epsc_b], w=[mv.B], bias=self.epsc, scale=1.0)
            self.recip(mv.t[:, :, 3:4], mv.t[:, :, 2:3], r=[mv.B], w=[mv.B])
            self.stt(nmr.t[:], mv.t[:, :, 0:1], -1.0, mv.t[:, :, 3:4], ALU.mult, ALU.mult, r=[mv.B], w=[nmr.B])
            for j in range(NT):
                b = j % 4
                g8 = j // 8
                self.act(xh.t[:, b, :], hs.t[:, j, :], AF.Identity, r=[hs.b[j], mv.B, nmr.B], w=[xh.b[b]],
                         scale=mv.t[:, j, 3:4], bias=nmr.t[:, j, :])
                self.tt(xh.t[:, b, :], xh.t[:, b, :], self.CF(lb + LP_MNG + hd * 128, 128), ALU.mult,
                        r=[xh.b[b], self.cf.B], w=[xh.b[b]], eng="pool")
                self.tt(hmt.t[:, b, :], xh.t[:, b, :], sigo.t[:, j, :], ALU.mult, r=[xh.b[b], sigo.B], w=[hmt.b[b]],
                        eng="pool")
                self.tr(psb[g8][:, (j % 8) * 128:(j % 8 + 1) * 128], hmt.t[:, b, :], self.CB(CB_ID, 128),
                        r=[hmt.b[b], self.cb.B], w=[pbb[g8]])
                if j % 8 == 7:
                    self.cp(hmT.t[:, hd, g8 * 1024:(g8 + 1) * 1024], psb[g8][:, :], r=[pbb[g8]], w=[hmT.B], eng="dve")
        sc.close()
        self.dump("hmT", hmT.t[:], [128, 4, S_LEN], BF16)
        self.check_stop("M1")

        sc = Scope(self)
        wd = sc.sb("wd", [128, 2, 8, 384], BF16, nb=2)
        qA = sc.sb("qA", [128, 2, S_LEN], BF16, nb=2)
        kA = sc.sb("kA", [128, 2, 2, S_LEN], BF16)
        Vd = sc.sb("Vd", [128, NT, 129], BF16)
        PT = sc.sb("PT", [128, 3, 512], BF16, nb=3)
        lm = sc.sb("lm", [128, 136], F32)
        glr = sc.sb("glr", [128, 128], F32)
        rr = sc.sb("rr", [128, 2, 8], F32, nb=2)
        t1 = sc.sb("t1", [128, 2, 128], F32, nb=2)
        ob = sc.sb("ob", [128, 2, 128], F32, nb=2)
        jk = sc.sb("jk", [128, 2, 128], F32, nb=2)
        odt = sc.sb("odt", [128, 2, 128], BF16, nb=2)
        self.memset(kA.t[32:64, :, 1, :], 0.0, w=[kA.B])
        self.memset(qA.t[32:64, 1, :], 0.0, w=[qA.b[1]])
        for v in range(2):
            S.dma("sp", kA.t[64:96, v, 0, :], self.kaug[v], kA.B, writes=[kA.B])
            S.dma("sp", kA.t[0:32, v, 1, :], self.kaug[v], kA.B, writes=[kA.B])
        self.memset(Vd.t[:, :, 128:129], 1.0, w=[Vd.B])
        lamc = lb + LP_LAM
        nc_ = self.nc
        for i2 in range(2):
            self.tt(lm.t[:, 0:64], self.CF(lamc + i2 * 128, 64), self.CF(lamc + i2 * 128 + 64, 64), ALU.mult,
                    r=[self.cf.B, lm.B], w=[lm.B])
            self.S.op("dve", lambda: nc_.vector.reduce_sum(out=lm.t[:, 64 + i2:65 + i2], in_=lm.t[:, 0:64], axis=AX.X),
                      [lm.B], [lm.B])
        self.act(lm.t[:, 66:68], lm.t[:, 64:66], AF.Exp, r=[lm.B], w=[lm.B])
        self.tt(lm.t[:, 68:69], lm.t[:, 66:67], lm.t[:, 67:68], ALU.subtract, r=[lm.B], w=[lm.B])
        self.tsc(lm.t[:, 69:70], lm.t[:, 68:69], float(lam_init), None, ALU.add, r=[lm.B], w=[lm.B])
        lamf = lm.t[:, 69:70]
        self.tsc(glr.t[:], self.CF(lb + LP_DNG, 128), float(1.0 - lam_init), None, ALU.mult, r=[self.cf.B], w=[glr.B])
        def load_wd(h_):
            for i3, off in enumerate((OFF_DQ, OFF_DK, OFF_DV)):
                c0 = off + h_ * 128
                S.dma("pool", wd.t[:, h_ % 2, :, i3 * 128:(i3 + 1) * 128], win_l[:, :, c0:c0 + 128], wd.b[h_ % 2],
                      writes=[wd.b[h_ % 2]])

        load_wd(0)
        for hd in range(4):
            S.dma("sp", qA.t[64:96, 0, :], self.qaug[hd], qA.b[0], writes=[qA.b[0]])
            S.dma("sp", qA.t[0:32, 1, :], self.qaug[hd], qA.b[1], writes=[qA.b[1]])
            for n in range(4):
                ns = slice(n * 512, (n + 1) * 512)
                bk = n % 2
                for kc in range(8):
                    self.mm(ps[bk][:, :], wd.t[:, hd % 2, kc, 0:128], hT.t[:, kc, ns], kc == 0, kc == 7,
                            r=[wd.b[hd % 2]], w=[pb[bk]])
                self.act(qA.t[0:64, 0, ns], ps[bk][0:64, :], AF.Copy, r=[pb[bk]], w=[qA.b[0]], scale=0.125)
                self.act(qA.t[64:128, 1, ns], ps[bk][64:128, :], AF.Copy, r=[pb[bk]], w=[qA.b[1]], scale=0.125)
                bk = 2 + n % 2
                for kc in range(8):
                    self.mm(ps[bk][:, :], wd.t[:, hd % 2, kc, 128:256], hT.t[:, kc, ns], kc == 0, kc == 7,
                            r=[wd.b[hd % 2]], w=[pb[bk]])
                self.cp(kA.t[0:64, 0, 0, ns], ps[bk][0:64, :], r=[pb[bk]], w=[kA.B], eng="act")
                self.cp(kA.t[0:64, 1, 0, ns], ps[bk][0:64, :], r=[pb[bk]], w=[kA.B], eng="dve")
                self.cp(kA.t[64:128, 0, 1, ns], ps[bk][64:128, :], r=[pb[bk]], w=[kA.B], eng="dve")
                self.cp(kA.t[64:128, 1, 1, ns], ps[bk][64:128, :], r=[pb[bk]], w=[kA.B], eng="act")
            for j in range(NT):
                bk = 4 + j % 2
                for kc in range(8):
                    self.mm(ps[bk][:, 0:128], hT.t[:, kc, j * 128:(j + 1) * 128], wd.t[:, hd % 2, kc, 256:384],
                            kc == 0, kc == 7, r=[wd.b[hd % 2]], w=[pb[bk]])
                self.cp(Vd.t[:, j, 0:128], ps[bk][:, 0:128], r=[pb[bk]], w=[Vd.B], eng="act")
            if hd + 1 < 4:
                load_wd(hd + 1)
            slope = 2.0 ** (-2.0 * (hd + 1))
            wmax = int(math.ceil(96.0 / slope / 128.0)) + 1
            iters = [(qt, kb) for qt in range(8) for kb in range(NT)
                     if min(abs(2 * qt + i - kb) for i in range(2)) < wmax]
            kept = {qt: [kb for (q_, kb) in iters if q_ == qt] for qt in range(8)}

            qkb = [0, 1, 6]

            def emit_qk(it):
                qt, kb = iters[it]
                bank = qkb[it % 3]
                ks = slice(kb * 128, (kb + 1) * 128)
                for half in range(2):
                    rel = ["a" if (2 * qt + i) > kb else ("b" if (2 * qt + i) < kb else "d") for i in range(2)]
                    kr = 96 if half == 0 else 128
                    if rel[0] == rel[1] and rel[0] != "d":
                        var = 0 if rel[0] == "a" else 1
                        self.mm(ps[bank][:, half * 256:(half + 1) * 256], kA.t[0:kr, var, half, ks],
                                qA.t[0:kr, half, qt * 256:(qt + 1) * 256], True, True, r=[kA.B, qA.b[half]],
                                w=[pb[bank]])
                    else:
                        for i in range(2):
                            var = 1 if rel[i] == "b" else 0
                            o_ = ps[bank][:, half * 256 + i * 128:half * 256 + (i + 1) * 128]
                            qs = slice((2 * qt + i) * 128, (2 * qt + i + 1) * 128)
                            self.mm(o_, kA.t[0:kr, var, half, ks], qA.t[0:kr, half, qs], True, rel[i] != "d",
                                    r=[kA.B, qA.b[half]], w=[pb[bank]])
                            if rel[i] == "d":
                                self.mm(o_, self.CB(CB_ID, 128), self.CB(CB_CORR + hd * 128, 128), False, True,
                                        w=[pb[bank]])
                self.act(PT.t[:, it % 3, :], ps[bank][:, :], AF.Exp, r=[pb[bank]], w=[PT.b[it % 3]])

            def emit_pv(it):
                qt, kb = iters[it]
                pbuf = it % 3
                for half in range(2):
                    for i in range(2):
                        pv = 2 + half * 2 + i
                        self.mm(ps[pv][:, 0:129], PT.t[:, pbuf, half * 256 + i * 128:half * 256 + (i + 1) * 128],
                                Vd.t[:, kb, :], kb == kept[qt][0], kb == kept[qt][-1], r=[PT.b[pbuf], Vd.B], w=[pb[pv]])
                if kb != kept[qt][-1]:
                    return
                for i in range(2):
                    b = i
                    p0, p1 = ps[2 + i], ps[4 + i]
                    self.recip(rr.t[:, b, 0:1], p0[:, 128:129], r=[pb[2 + i]], w=[rr.b[b]])
                    self.recip(rr.t[:, b, 1:2], p1[:, 128:129], r=[pb[4 + i]], w=[rr.b[b]])
                    self.tt(rr.t[:, b, 2:3], rr.t[:, b, 1:2], lamf, ALU.mult, r=[rr.b[b], lm.B], w=[rr.b[b]])
                    self.tsc(t1.t[:, b, :], p1[:, 0:128], rr.t[:, b, 2:3], None, ALU.mult, r=[pb[4 + i], rr.b[b]],
                             w=[t1.b[b]])
                    self.stt(ob.t[:, b, :], p0[:, 0:128], rr.t[:, b, 0:1], t1.t[:, b, :], ALU.mult, ALU.subtract,
                             r=[pb[2 + i], rr.b[b], t1.b[b]], w=[ob.b[b]])
                for i in range(2):
                    qb = 2 * qt + i
                    b = i
                    g8 = qb // 8
                    nc_ = self.nc
                    self.S.op("dve", lambda: nc_.vector.scalar_tensor_tensor(
                        out=jk.t[:, b, :], in0=ob.t[:, b, :], scalar=1.0, in1=ob.t[:, b, :],
                        op0=ALU.mult, op1=ALU.mult, accum_out=rr.t[:, b, 3:4]), [ob.b[b]], [jk.b[b], rr.b[b]])
                    self.act(rr.t[:, b, 4:5], rr.t[:, b, 3:4], AF.Ln, r=[rr.b[b], self.epsc_b], w=[rr.b[b]], bias=self.epsc,
                             scale=1.0 / 128)
                    self.act(rr.t[:, b, 5:6], rr.t[:, b, 4:5], AF.Exp, r=[rr.b[b]], w=[rr.b[b]], scale=-0.5)
                    self.stt(odt.t[:, b, :], ob.t[:, b, :], rr.t[:, b, 5:6], glr.t[:], ALU.mult, ALU.mult,
                             r=[ob.b[b], rr.b[b], glr.B], w=[odt.b[b]])
                    self.tr(psb[1][:, (qb % 8) * 128:(qb % 8 + 1) * 128], odt.t[:, b, :], self.CB(CB_ID, 128),
                            r=[odt.b[b], self.cb.B], w=[pbb[1]])
                    if qb % 8 == 7:
                        self.cp(odT.t[:, hd, g8 * 1024:(g8 + 1) * 1024], psb[1][:, :], r=[pbb[1]], w=[odT.B],
                                eng="dve")

            LA = 2
            for n_ in range(len(iters) + LA):
                if n_ < len(iters):
                    emit_qk(n_)
                if n_ >= LA:
                    emit_pv(n_ - LA)
        sc.close()
        self.dump("odT", odT.t[:], [128, 4, S_LEN], BF16)
        self.check_stop("M3")

        sc = Scope(self)
        uT = sc.sb("uT", [128, 8, S_LEN], BF16)
        wga = sc.sb("wga", [128, 2, 2, 8, 128], BF16, nb=2)
        wua = sc.sb("wua", [128, 2, 2, 4, 128], BF16, nb=2)
        sg = sc.sb("sg", [128, 2, 512], F32, nb=2)
        tu = sc.sb("tu", [128, 2, 512], F32, nb=2)
        wupa = self.w_up_a[l].rearrange("(kc p) c -> p kc c", p=128)
        wupb = self.w_up_b[l].rearrange("(kc p) c -> p kc c", p=128)
        for ec in range(8):
            es = slice(ec * 128, (ec + 1) * 128)
            for ab in range(2):
                c0 = OFF_GATE + ab * D + ec * 128
                S.dma("pool", wga.t[:, ec % 2, ab, :, :], win_l[:, :, c0:c0 + 128], wga.b[ec % 2], writes=[wga.b[ec % 2]])
                S.dma("pool", wua.t[:, ec % 2, ab, :, :], (wupa if ab == 0 else wupb)[:, :, es], wua.b[ec % 2],
                      writes=[wua.b[ec % 2]])
            for n in range(4):
                ns = slice(n * 512, (n + 1) * 512)
                b4 = (n % 2) * 4
                for ab in range(2):
                    src = hmT if ab == 0 else odT
                    for kc in range(4):
                        self.mm(ps[b4 + ab][:, :], wua.t[:, ec % 2, ab, kc, :], src.t[:, kc, ns], kc == 0, kc == 3,
                                r=[wua.b[ec % 2]], w=[pb[b4 + ab]])
                    for kc in range(8):
                        self.mm(ps[b4 + 2 + ab][:, :], wga.t[:, ec % 2, ab, kc, :], hT.t[:, kc, ns], kc == 0, kc == 7,
                                r=[wga.b[ec % 2]], w=[pb[b4 + 2 + ab]])
                for ab in range(2):
                    self.act(sg.t[:, ab, :], ps[b4 + 2 + ab][:, :], AF.Sigmoid, r=[pb[b4 + 2 + ab]], w=[sg.b[ab]])
                    self.tt(tu.t[:, ab, :], ps[b4 + ab][:, :], sg.t[:, ab, :], ALU.mult, r=[pb[b4 + ab], sg.b[ab]],
                            w=[tu.b[ab]])
                self.tt(uT.t[:, ec, ns], tu.t[:, 0, :], tu.t[:, 1, :], ALU.add, r=[tu.b[0], tu.b[1]], w=[uT.B])
        S.barrier()
        self.dump("uT", uT.t[:], [128, 8, S_LEN], BF16)
        wo = sc.sb("wo", [128, 8, D], BF16)
        wout_l = self.w_out[l].rearrange("(kc p) c -> p kc c", p=128)
        for half in range(2):
            S.dma("pool", wo.t[:, :, half * 512:(half + 1) * 512], wout_l[:, :, half * 512:(half + 1) * 512], wo.B,
                  writes=[wo.B])
        xin = sc.sb("xin2", [128, 2, D], F32, nb=2)
        xo = sc.sb("xo", [128, 2, D], F32, nb=2)
        grow2 = sc.sb("grow2", [128, D], F32)
        S.dma("sp", grow2.t[:], self.grows[2 + l], grow2.B, writes=[grow2.B])
        xn = sc.sb("xn2", [128, 2, D], F32, nb=2)
        h2b = sc.sb("h2b", [128, 2, D], BF16, nb=2)
        h2T = sc.sb("h2T", [128, 2, 8, 128], F32, nb=2)
        st = sc.sb("st2", [128, NT, 4], F32, nb=NT)
        sm = sc.sb("sm", [128, NT, 4], F32, nb=NT)
        lg = sc.sb("lg", [128, 2, 16], F32, nb=2)
        aff2 = self.aff2
        def p1(j):
            b = j % 2
            src, sbuf = xsrc(j)
            S.dma("sp", xin.t[:, b, :], src, xin.b[b], reads=[sbuf] if sbuf else [], writes=[xin.b[b]])
            for half in range(2):
                bk = (j % 2) * 2 + half
                hs_ = slice(half * 512, (half + 1) * 512)
                for ec in range(8):
                    self.mm(ps[bk][:, :], uT.t[:, ec, j * 128:(j + 1) * 128], wo.t[:, ec, hs_], ec == 0, ec == 7,
                            r=[wo.B], w=[pb[bk]])
                self.tt(xo.t[:, b, hs_], ps[bk][:, :], xin.t[:, b, hs_], ALU.add, r=[pb[bk], xin.b[b]], w=[xo.b[b]])
            S.dma("sp", self.xres[s][j * 128:(j + 1) * 128, :], xo.t[:, b, :], xo.b[b], reads=[xo.b[b]],
                  writes=[self.xres_b[s][j]])

        def p2(j):
            b = j % 2
            self.rms_stats(xo.t[:, b, :], xo.b[b], xn.t[:, b, :], xn.b[b], st.t[:, j, 0:1], st.t[:, j, 1:2],
                           st.t[:, j, 2:3], st.b[j])
            self.stt(xn.t[:, b, :], xo.t[:, b, :], st.t[:, j, 2:3], grow2.t[:], ALU.mult, ALU.mult,
                     r=[xo.b[b], st.b[j], grow2.B], w=[xn.b[b]])
            self.cp(h2b.t[:, b, :], xn.t[:, b, :], r=[xn.b[b]], w=[h2b.b[b]], eng="act")
            S.dma("sp", self.h2d[s][j * 128:(j + 1) * 128, :], h2b.t[:, b, :], h2b.b[b], reads=[h2b.b[b]],
                  writes=[self.h2d_b[s][j]])

        def p3(j):
            b = j % 2
            for kc in range(8):
                bk = 4 if kc < 4 else 5
                self.tr(ps[bk][:, (kc % 4) * 128:(kc % 4 + 1) * 128], xn.t[:, b, kc * 128:(kc + 1) * 128],
                        self.CF(CF_ID, 128), r=[xn.b[b], self.cf.B], w=[pb[bk]])
            self.cp(h2T.t[:, b, 0:4, :], ps[4][:, :].rearrange("p (k t) -> p k t", k=4), r=[pb[4]], w=[h2T.b[b]],
                    eng="act")
            self.cp(h2T.t[:, b, 4:8, :], ps[5][:, :].rearrange("p (k t) -> p k t", k=4), r=[pb[5]], w=[h2T.b[b]],
                    eng="dve")
            lbk = 6 + j % 2
            for kc in range(8):
                self.mm(ps[lbk][:, 0:16], h2T.t[:, b, kc, :], self.CF(lb + LP_WR + kc * 16, 16), kc == 0, kc == 7,
                        r=[h2T.b[b], self.cf.B], w=[pb[lbk]])

        def p4(j):
            b = j % 2
            lbk = 6 + j % 2
            nc_ = self.nc
            self.S.op("dve", lambda: nc_.vector.reduce_max(out=sm.t[:, j, 0:1], in_=ps[lbk][:, 0:16], axis=AX.X),
                      [pb[lbk]], [sm.b[j]])
            self.tsc(sm.t[:, j, 1:2], sm.t[:, j, 0:1], -1.0, None, ALU.mult, r=[sm.b[j]], w=[sm.b[j]])
            self.act(lg.t[:, b, :], ps[lbk][:, 0:16], AF.Exp, r=[pb[lbk], sm.b[j]], w=[lg.b[b], sm.b[j]],
                     bias=sm.t[:, j, 1:2], accum_out=sm.t[:, j, 2:3])
            self.recip(sm.t[:, j, 3:4], sm.t[:, j, 2:3], r=[sm.b[j]], w=[sm.b[j]])
            self.tsc(aff2.t[:, j, s * 16:(s + 1) * 16], lg.t[:, b, :], sm.t[:, j, 3:4], None, ALU.mult,
                     r=[lg.b[b], sm.b[j]], w=[aff2.B])

        for i in range(NT + 3):
            if i < NT:
                p1(i)
            if 0 <= i - 1 < NT:
                p2(i - 1)
            if 0 <= i - 2 < NT:
                p3(i - 2)
            if 0 <= i - 3 < NT:
                p4(i - 3)
        sc.close()
        if "xmid" in self.dbg:
            self.dump("xmid", self.xres[s], [S_LEN, D], F32)
        self.check_stop("M4")
        M.close()

    def phase_E(self, l):
        nc, S = self.nc, self.S
        ps, pb, psb, pbb = self.ps, self.pb, self.psb, self.pbb
        lb = CF_L0 + l * LP
        last = l == DEPTH - 1
        nseq = self.nseq
        E = Scope(self)
        epsc = E.sb("epsc", [128, 1], F32)
        self.memset(epsc.t[:], EPS, w=[epsc.B])
        self.epsc = epsc.t[:, 0:1]
        self.epsc_b = epsc.B
        aff2 = self.aff2
        mask2 = E.sb("mask2", [128, NT, 32], F32)
        gsel2 = E.sb("gsel2", [128, NT, 32], F32)
        rank2 = E.sb("rank2", [128, NT, 32], F32)
        self.dump("aff2", aff2.t[:], [128, NT, 32], F32)
        self.check_stop("Eroute")

        sc = Scope(self)
        affT = sc.sb("affT", [128, 512], F32)
        mask128 = sc.sb("mask128", [128, 512], F32)
        junk = sc.sb("junk", [128, 512], BF16)
        thr = sc.sb("thr", [128, 4], F32)
        maskb = sc.sb("maskb", [128, NT, 32], BF16)
        Tsb = sc.sb("Tsb", [128, NT, 32], F32)
        cum = sc.sb("cum", [128, NT, 32], F32)
        for g4 in range(4):
            self.tr(ps[0][:, g4 * 128:(g4 + 1) * 128], aff2.t[:, g4 * 4:(g4 + 1) * 4, :].rearrange("p j c -> p (j c)"),
                    self.CF(CF_ID, 128), r=[aff2.B, self.cf.B], w=[pb[0]])
        self.cp(affT.t[:], ps[0][:, :], r=[pb[0]], w=[affT.B], eng="dve")
        self.memset(thr.t[:], 0.0, w=[thr.B])
        self.check_stop("Es1")
        for it in range(26):
            step = 2.0 ** (-(it + 1))
            self.tsc(thr.t[:, 1:2], thr.t[:, 0:1], step, None, ALU.add, r=[thr.B], w=[thr.B])
            self.tsc(junk.t[:], affT.t[:], thr.t[:, 1:2], None, ALU.is_ge, ALU.add, r=[affT.B, thr.B],
                     w=[junk.B, thr.B], accum_out=thr.t[:, 2:3])
            self.mm(ps[1][:, 0:2], self.CF(CF_BM, 128), thr.t[:, 2:4], True, True, r=[thr.B, self.cf.B], w=[pb[1]])
            self.tsc(thr.t[:, 3:4], ps[1][:, 0:1], CAP - 0.5, thr.t[:, 1:2], ALU.is_ge, ALU.mult, r=[thr.B, pb[1]],
                     w=[thr.B])
            self.tt(thr.t[:, 0:1], thr.t[:, 0:1], thr.t[:, 3:4], ALU.max, r=[thr.B], w=[thr.B])
            if it == 0:
                self.check_stop("Es2")
        self.check_stop("Es3")
        self.tsc(mask128.t[:], affT.t[:], thr.t[:, 0:1], None, ALU.is_ge, r=[affT.B, thr.B], w=[mask128.B])
        for g4 in range(4):
            self.tr(ps[4][:, g4 * 128:(g4 + 1) * 128], mask128.t[:, g4 * 128:(g4 + 1) * 128], self.CF(CF_ID, 128),
                    r=[mask128.B, self.cf.B], w=[pb[4]])
        self.check_stop("Es4")
        m2f = mask2.t[:].rearrange("p j c -> p (j c)")
        self.cp(m2f, ps[4][:, :], r=[pb[4]], w=[mask2.B], eng="dve")
        self.cp(maskb.t[:].rearrange("p j c -> p (j c)"), m2f, r=[mask2.B], w=[maskb.B], eng="act")
        self.tt(gsel2.t[:], aff2.t[:], mask2.t[:], ALU.mult, r=[aff2.B, mask2.B], w=[gsel2.B])
        self.check_stop("Es5")
        mbf = maskb.t[:].rearrange("p j c -> p (j c)")
        self.mm(ps[5][:, :], self.CB(CB_TS, 128), mbf, True, True, r=[maskb.B, self.cb.B], w=[pb[5]])
        self.mm(ps[6][:, :], self.CB(CB_ONE, 128), mbf, True, True, r=[maskb.B, self.cb.B], w=[pb[6]])
        self.check_stop("Es6")
        self.cp(Tsb.t[:].rearrange("p j c -> p (j c)"), ps[6][:, :], r=[pb[6]], w=[Tsb.B], eng="act")
        self.memset(cum.t[:, 0, :], 0.0, w=[cum.B])
        for j in range(1, NT):
            self.tt(cum.t[:, j, :], cum.t[:, j - 1, :], Tsb.t[:, j - 1, :], ALU.add, r=[cum.B, Tsb.B], w=[cum.B])
        self.tt(rank2.t[:].rearrange("p j c -> p (j c)"), ps[5][:, :], cum.t[:].rearrange("p j c -> p (j c)"), ALU.add,
                r=[pb[5], cum.B], w=[rank2.B])
        self.check_stop("Es7")
        sc.close()
        self.dump("mask2", mask2.t[:], [128, NT, 32], F32)
        self.dump("rank2", rank2.t[:], [128, NT, 32], F32)
        self.check_stop("Esel")

        I32 = mybir.dt.int32
        idx_all = E.sb("idx_all", [128, NSEQ, 2 * NEXP], I32)
        g_all = E.sb("g_all", [128, NSEQ, 2 * NEXP], F32)
        sc = Scope(self)
        R = sc.sb("R", [128, NT, 32, 4], BF16)
        tokc = self.CB(CB_TOK, 2 * NT).rearrange("p (j o k) -> p j o k", j=NT, o=1)
        self.cp(R.t[:, :, :, 0:2], tokc.to_broadcast([128, NT, 32, 2]), r=[self.cb.B], w=[R.B], eng="dve")
        self.cp(R.t[:, :, :, 2:3], aff2.t[:].rearrange("p j (c o) -> p j c o", o=1), r=[aff2.B], w=[R.B], eng="dve")
        self.tt(R.t[:, :, :, 3:4], aff2.t[:].rearrange("p j (c o) -> p j c o", o=1), R.t[:, :, :, 2:3], ALU.subtract,
                r=[aff2.B, R.B], w=[R.B])
        Sel = sc.sb("Sel", [128, 2, NT, CAP], BF16, nb=2)
        ig = sc.sb("ig", [128, 2 * NEXP, 4], F32)
        igf = sc.sb("igf", [128, 2 * NEXP], F32)
        for s in range(nseq):
            for e in range(NEXP):
                col = s * 16 + e
                b = e % 2
                for j in range(NT):
                    self.tsc(Sel.t[:, b, j, :], self.CB(CB_IOTA, CAP), rank2.t[:, j, col:col + 1],
                             mask2.t[:, j, col:col + 1], ALU.is_equal, ALU.mult, r=[self.cb.B, rank2.B, mask2.B],
                             w=[Sel.b[b]])
                for ch in range(2):
                    i = e * 2 + ch
                    for j in range(NT):
                        self.mm(ps[s][:, i * 4:(i + 1) * 4], Sel.t[:, b, j, ch * 128:(ch + 1) * 128], R.t[:, j, col, :],
                                j == 0, j == NT - 1, r=[Sel.b[b], R.B], w=[pb[s]])
            self.cp(ig.t[:].rearrange("p i k -> p (i k)"), ps[s][:, 0:128], r=[pb[s]], w=[ig.B], eng="dve")
            self.tt(igf.t[:], ig.t[:, :, 0], ig.t[:, :, 1], ALU.add, r=[ig.B], w=[igf.B])
            self.cp(idx_all.t[:, s, :], igf.t[:], r=[igf.B], w=[idx_all.B], eng="dve")
            self.tt(g_all.t[:, s, :], ig.t[:, :, 2], ig.t[:, :, 3], ALU.add, r=[ig.B], w=[g_all.B])
        sc.close()
        self.dump("idx_all", idx_all.t[:], [128, NSEQ, 2 * NEXP], I32)
        self.dump("g_all", g_all.t[:], [128, NSEQ, 2 * NEXP], F32)
        self.check_stop("Eidx")

        sc = Scope(self)
        xtok = sc.sb("xtok", [128, 2, 4, D], BF16, nb=2)
        xeb = sc.sb("xeb", [128, 2, 8, 512], BF16, nb=2)
        wg = sc.sb("wg", [128, 2, 8, 512], BF16, nb=2)
        wu = sc.sb("wu", [128, 2, 8, 512], BF16, nb=2)
        wdn = sc.sb("wdn", [128, 2, 16, 512], BF16, nb=2)
        hid = sc.sb("hid", [128, 16, 512], BF16, nb=16)
        sgl = sc.sb("sgl", [128, 2, 512], F32, nb=2)
        yst = sc.sb("yst", [128, 2, 4, D], F32, nb=2)
        self.memset(xeb.t[:], 0.0, w=[xeb.b[0], xeb.b[1]])
        IO = bass.IndirectOffsetOnAxis

        def emit_gather(e):
            be = e % 2
            for cc in range(4):
                s2, ch = cc // 2, cc % 2
                if s2 < nseq:
                    S.idma(xtok.t[:, be, cc, :], self.h2d[s2], xtok.b[be],
                           in_off=IO(ap=idx_all.t[:, s2, e * 2 + ch:e * 2 + ch + 1], axis=0),
                           reads=self.h2d_b[s2] + [idx_all.B], writes=[xtok.b[be]])

        def emit_scatter(e):
            be = e % 2
            for cc in range(4):
                s2, ch = cc // 2, cc % 2
                if s2 < nseq:
                    S.idma(self.xres[s2], yst.t[:, be, cc, :], yst.b[be],
                           out_off=IO(ap=idx_all.t[:, s2, e * 2 + ch:e * 2 + ch + 1], axis=0),
                           reads=[yst.b[be], idx_all.B], writes=self.xres_b[s2], compute_op=ALU.add)

        def emit_xpose(e):
            be = e % 2
            for cc in range(4):
                if cc // 2 >= nseq:
                    continue
                g = cc % 2
                for kc in range(8):
                    self.tr(psb[g][:, kc * 128:(kc + 1) * 128], xtok.t[:, be, cc, kc * 128:(kc + 1) * 128],
                            self.CB(CB_ID, 128), r=[xtok.b[be], self.cb.B], w=[pbb[g]])
                self.cp(xeb.t[:, be, :, cc * 128:(cc + 1) * 128], psb[g][:, :].rearrange("p (k c) -> p k c", k=8),
                        r=[pbb[g]], w=[xeb.b[be]], eng="act" if cc % 2 else "dve")

        emit_gather(0)
        for e in range(NEXP):
            be = e % 2
            wge = self.w_gate_e[l, e].rearrange("(kc p) f -> p kc f", p=128)
            wue = self.w_up_e[l, e].rearrange("(kc p) f -> p kc f", p=128)
            wde = self.w_down_e[l, e].rearrange("(fc p) d -> p fc d", p=128)
            emit_xpose(e)
            if e + 1 < NEXP:
                emit_gather(e + 1)
            for fg in range(4):
                wb = (e * 4 + fg) % 2
                fs = slice(fg * 512, (fg + 1) * 512)
                S.dma("pool", wg.t[:, wb, :, :], wge[:, :, fs], wg.b[wb], writes=[wg.b[wb]])
                S.dma("pool", wu.t[:, wb, :, :], wue[:, :, fs], wu.b[wb], writes=[wu.b[wb]])
                if fg == 1 and e >= 1:
                    emit_scatter(e - 1)
                for fc in range(4):
                    f = fg * 4 + fc
                    pk = (f % 2) * 2
                    for kc in range(8):
                        self.mm(ps[pk][:, :], wg.t[:, wb, kc, fc * 128:(fc + 1) * 128], xeb.t[:, be, kc, :], kc == 0,
                                kc == 7, r=[wg.b[wb], xeb.b[be]], w=[pb[pk]])
                    for kc in range(8):
                        self.mm(ps[pk + 1][:, :], wu.t[:, wb, kc, fc * 128:(fc + 1) * 128], xeb.t[:, be, kc, :], kc == 0,
                                kc == 7, r=[wu.b[wb], xeb.b[be]], w=[pb[pk + 1]])
                    self.act(sgl.t[:, f % 2, :], ps[pk][:, :], AF.Silu, r=[pb[pk]], w=[sgl.b[f % 2]])
                    self.tt(hid.t[:, f, :], ps[pk + 1][:, :], sgl.t[:, f % 2, :], ALU.mult, r=[pb[pk + 1], sgl.b[f % 2]],
                            w=[hid.b[f]])
            for dh in range(2):
                wb2 = (e * 2 + dh) % 2
                S.dma("pool", wdn.t[:, wb2, :, :], wde[:, :, dh * 512:(dh + 1) * 512], wdn.b[wb2], writes=[wdn.b[wb2]])
                for cc in range(4):
                    bank = 4 + cc % 2
                    s2, ch = cc // 2, cc % 2
                    for fc in range(16):
                        self.mm(ps[bank][:, :], hid.t[:, fc, cc * 128:(cc + 1) * 128], wdn.t[:, wb2, fc, :], fc == 0,
                                fc == 15, r=[hid.b[fc], wdn.b[wb2]], w=[pb[bank]])
                    gcol = g_all.t[:, min(s2, nseq - 1), e * 2 + ch:e * 2 + ch + 1]
                    if cc % 2:
                        self.act(yst.t[:, be, cc, dh * 512:(dh + 1) * 512], ps[bank][:, :], AF.Copy,
                                 r=[pb[bank], g_all.B], w=[yst.b[be]], scale=gcol)
                    else:
                        self.tsc(yst.t[:, be, cc, dh * 512:(dh + 1) * 512], ps[bank][:, :], gcol, None, ALU.mult,
                                 r=[pb[bank], g_all.B], w=[yst.b[be]])
        emit_scatter(NEXP - 1)
        sc.close()
        self.check_stop("Effn")

        if last:
            sc = Scope(self)
            grow = sc.sb("grow", [128, D], F32)
            S.dma("sp", grow.t[:], self.grows[4], grow.B, writes=[grow.B])
            xin = sc.sb("xin", [128, 2, D], F32, nb=2)
            xf = sc.sb("xf", [128, 2, D], F32, nb=2)
            st = sc.sb("st", [128, NSEQ * NT, 4], F32, nb=NSEQ * NT)
            for s in range(nseq):
                for j in range(NT):
                    b = j % 2
                    k_ = s * NT + j
                    S.dma("sp", xin.t[:, b, :], self.xres[s][j * 128:(j + 1) * 128, :], xin.b[b],
                          reads=[self.xres_b[s][j]], writes=[xin.b[b]])
                    self.rms_stats(xin.t[:, b, :], xin.b[b], xf.t[:, b, :], xf.b[b], st.t[:, k_, 0:1], st.t[:, k_, 1:2],
                                   st.t[:, k_, 2:3], st.b[k_])
                    self.stt(xf.t[:, b, :], xin.t[:, b, :], st.t[:, k_, 2:3], grow.t[:], ALU.mult, ALU.mult,
                             r=[xin.b[b], st.b[k_], grow.B], w=[xf.b[b]])
                    S.dma("sp", self.y[s, j * 128:(j + 1) * 128, :], xf.t[:, b, :], xf.b[b], reads=[xf.b[b]])
            sc.close()
        E.close()
        if "xe%d" % l in self.dbg:
            self.dump("xe%d" % l, self.xres[0] if not last else self.y[0], [S_LEN, D], F32)
        self.check_stop("E%d" % l)


def host_consts(inp):
    cf = np.zeros((128, NCF), np.float32)
    p = np.arange(128)
    cf[:, CF_ID:CF_ID + 128] = np.eye(128)
    cf[:, CF_TL:CF_TL + 128] = (p[:, None] <= p[None, :])
    cf[:, CF_TU:CF_TU + 128] = (p[:, None] >= p[None, :])
    cf[:, CF_ONE:CF_ONE + 128] = 1.0
    cf[:, CF_IOTA:CF_IOTA + 256] = np.arange(256)[None, :]
    for l in range(DEPTH):
        lb = CF_L0 + l * LP
        cf[:, lb + LP_BMG:lb + LP_BMG + 256] = np.tile(inp["b_mgate"][l], NT)[None, :]
        cw = inp["conv_w"][l]
        cf[:, lb + LP_CW:lb + LP_CW + 40] = cw.reshape(5, 8, 128).transpose(2, 1, 0).reshape(128, 40)
        cf[:, lb + LP_CB:lb + LP_CB + 8] = inp["conv_b"][l].reshape(8, 128).T
        cf[:, lb + LP_MNG:lb + LP_MNG + 512] = inp["mlstm_norm_g"][l][None, :]
        cf[:, lb + LP_DNG:lb + LP_DNG + 128] = inp["diff_norm_g"][l][None, :]
        cf[:, lb + LP_LAM:lb + LP_LAM + 256] = inp["diff_lam"][l].reshape(256)[None, :]
        cf[:, lb + LP_WR:lb + LP_WR + 128] = inp["w_router"][l].reshape(8, 128, 16).transpose(1, 0, 2).reshape(128, 128)
    cf[:, CF_BM:CF_BM + 128] = ((p[:, None] % 32) == (p[None, :] % 32))
    cb = np.zeros((128, NCB), np.float32)
    cb[:, CB_ID:CB_ID + 128] = np.eye(128)
    cb[:, CB_TL:CB_TL + 128] = (p[:, None] <= p[None, :])
    cb[:, CB_TU:CB_TU + 128] = (p[:, None] >= p[None, :])
    cb[:, CB_TS:CB_TS + 128] = (p[:, None] < p[None, :])
    cb[:, CB_ONE:CB_ONE + 128] = 1.0
    cb[:, CB_IOTA:CB_IOTA + CAP] = np.arange(CAP)[None, :]
    for j in range(NT):
        cb[:, CB_TOK + 2 * j] = 128.0 * j
        cb[:, CB_TOK + 2 * j + 1] = p
    pos = np.arange(S_LEN)
    hi, lo = (pos // 128).astype(np.float32), (pos % 128).astype(np.float32)
    kaug = np.zeros((2, 32, S_LEN), np.float32)
    kaug[0, 0], kaug[0, 1], kaug[0, 2], kaug[0, 3] = -1.0, -1.0, 128.0 * hi, lo
    kaug[1] = -kaug[0]
    qaug = np.zeros((4, 32, S_LEN), np.float32)
    for h in range(4):
        sl = 2.0 ** (-2.0 * (h + 1))
        qaug[h, 0], qaug[h, 1], qaug[h, 2], qaug[h, 3] = sl * 128.0 * hi, sl * lo, sl, sl
        kq = p[None, :].astype(np.float32) - p[:, None].astype(np.float32)
        cb[:, CB_CORR + h * 128:CB_CORR + (h + 1) * 128] = np.where(kq < 0, 2.0 * sl * kq, 0.0)
    grows = np.stack([np.broadcast_to(v[None, :], (128, D)) for v in
                      (inp["norm_mix_g"][0], inp["norm_mix_g"][1], inp["norm_ffn_g"][0], inp["norm_ffn_g"][1],
                       inp["norm_f_g"])]).astype(np.float32)
    bf = ml_dtypes.bfloat16
    return dict(cf=cf, cb=cb.astype(bf), grows=np.ascontiguousarray(grows), kaug=kaug.astype(bf), qaug=qaug.astype(bf))


def make_in_maps(inp, ncores=NCORES, use_moe=True):
    inp = {k: np.asarray(v) for k, v in inp.items()}
    hc = host_consts(inp)
    shared = dict(w_in=inp["w_in"], w_up_a=inp["w_up_a"], w_up_b=inp["w_up_b"], w_out=inp["w_out"], **hc)
    if use_moe:
        shared.update(w_gate_e=inp["w_gate_e"], w_up_e=inp["w_up_e"], w_down_e=inp["w_down_e"])
    maps = []
    for c in range(ncores):
        m = dict(shared)
        m["x"] = np.ascontiguousarray(inp["x"][c * NSEQ:(c + 1) * NSEQ])
        maps.append(m)
    return maps


def kernel(**inputs):
    k = K()
    nc = k.build()
    maps = make_in_maps(inputs)
    res = run_bass_kernel_spmd(nc, maps, core_ids=list(range(NCORES)))
    return np.concatenate([np.asarray(r["y"]) for r in res.results], axis=0).astype(np.float32)
```
